# Optimizing a Trainium2 kernel written in Bass

```python
import jax, jax.numpy as jnp
from jax import lax
import numpy as np

D_MODEL = 1024
BATCH = 4
SEQ = 4096
DEPTH = 2

CHUNK = 64
N_META = 16
Q_BLOCK = 128
A_HEADS = 8
A_KV_HEADS = 2
A_HEAD_DIM = 64
IDX_HEADS = 8
IDX_DIM = 32
TOPK_MAX = 256
A_Q = A_HEADS * A_HEAD_DIM
A_KV = A_KV_HEADS * A_HEAD_DIM
IDX_Q = IDX_HEADS * IDX_DIM
A_COLS = A_Q + 2 * A_KV + IDX_Q + IDX_DIM + IDX_HEADS
R_HEADS = 8
R_HEAD_DIM = 64
R_WIDTH = R_HEADS * R_HEAD_DIM
W_LORA = 64
A_LORA = 64
G_LORA = 128
R_COLS = 3 * R_WIDTH + W_LORA + A_LORA + G_LORA
GN_EPS = 64e-5
GATE_COLS = 2 * D_MODEL
IN_COLS = A_COLS + R_COLS + GATE_COLS
N_EXPERTS = 16
N_GROUPS = 4
EXPERTS_PER_GROUP = N_EXPERTS // N_GROUPS
TOP_K_EXPERTS = 2
D_EXPERT = 512
LN_EPS = 1e-5
ALPHA = (2 * DEPTH) ** 0.25
BETA = (8 * DEPTH) ** -0.25

kernel_name = 'hybrid_dsa_rwkv7_grouped_moe_deepnorm'


def layer_norm(x, g, b):
    xf = x.astype(jnp.float32)
    mu = jnp.mean(xf, axis=-1, keepdims=True)
    var = jnp.mean(jnp.square(xf - mu), axis=-1, keepdims=True)
    return ((xf - mu) * lax.rsqrt(var + LN_EPS) * g.astype(jnp.float32) + b.astype(jnp.float32)).astype(x.dtype)


def split_cols(u, sizes):
    outs, off = [], 0
    for s in sizes:
        outs.append(u[..., off:off + s])
        off += s
    return outs


def chunk_ids(n):
    p = jnp.arange(n)
    return jnp.where(p < N_META, 0, 1 + (p - N_META) // CHUNK)


def dsa_attention(q, k, v, qi, ki, wi, topk):
    B, L = q.shape[0], q.shape[1]
    n_blocks = -(-L // Q_BLOCK)
    Lp = n_blocks * Q_BLOCK
    pad = Lp - L
    padq = lambda a: jnp.pad(a, [(0, 0), (0, pad)] + [(0, 0)] * (a.ndim - 2))
    q, qi, wi = padq(q), padq(qi), padq(wi)
    cid_k = chunk_ids(L)
    cid_q = chunk_ids(Lp)
    scale = A_HEAD_DIM ** -0.5
    group = A_HEADS // A_KV_HEADS
    gather = jax.vmap(lambda a, ii: a[ii])

    def block(i):
        s0 = i * Q_BLOCK
        qb = lax.dynamic_slice_in_dim(q, s0, Q_BLOCK, axis=1)
        qib = lax.dynamic_slice_in_dim(qi, s0, Q_BLOCK, axis=1)
        wib = lax.dynamic_slice_in_dim(wi, s0, Q_BLOCK, axis=1)
        cq = lax.dynamic_slice_in_dim(cid_q, s0, Q_BLOCK)
        rel = jax.nn.relu(jnp.einsum('bqhd,bsd->bqhs', qib, ki)).astype(jnp.float32)
        idx_score = jnp.einsum('bqh,bqhs->bqs', wib.astype(jnp.float32), rel)
        admissible = cid_k[None, :] <= cq[:, None]
        idx_score = jnp.where(admissible[None], idx_score, -jnp.inf)
        _, sel = lax.top_k(idx_score, topk)
        ks = gather(k, sel)
        vs = gather(v, sel)
        valid = cid_k[sel] <= cq[None, :, None]
        qg = qb.reshape(B, Q_BLOCK, A_KV_HEADS, group, A_HEAD_DIM)
        s = jnp.einsum('bqngd,bqknd->bqngk', qg, ks).astype(jnp.float32) * scale
        s = jnp.where(valid[:, :, None, None, :], s, -jnp.inf)
        p = jax.nn.softmax(s, axis=-1).astype(vs.dtype)
        o = jnp.einsum('bqngk,bqknd->bqngd', p, vs)
        return o.reshape(B, Q_BLOCK, A_Q)

    out = lax.map(block, jnp.arange(n_blocks))
    out = jnp.moveaxis(out, 0, 1).reshape(B, Lp, A_Q)
    return out[:, :L]


def rwkv7_time_mix(u, mu, w0, w_up, a0, a_up, g_up, k_k, k_a, r_k, gn_g, gn_b):
    B, L, _ = u.shape
    dt = u.dtype
    u = u.astype(jnp.float32)
    u_prev = jnp.pad(u, ((0, 0), (1, 0), (0, 0)))[:, :-1]
    u = u + (u_prev - u) * mu.astype(jnp.float32)
    r, k, v, xw, xa, xg = split_cols(u, [R_WIDTH, R_WIDTH, R_WIDTH, W_LORA, A_LORA, G_LORA])
    f32 = lambda t: t.astype(jnp.float32)
    logw = -jax.nn.softplus(-(f32(w0) + jnp.tanh(xw) @ f32(w_up))) - 0.5
    decay = jnp.exp(-jnp.exp(logw))
    a = jax.nn.sigmoid(f32(a0) + xa @ f32(a_up))
    g = jax.nn.sigmoid(xg) @ f32(g_up)
    hd = lambda t: t.reshape(B, L, R_HEADS, R_HEAD_DIM)
    kk = hd(k * f32(k_k))
    kk = kk * lax.rsqrt(jnp.maximum(jnp.sum(kk * kk, axis=-1, keepdims=True), 1e-24))
    k = k * (1.0 + (a - 1.0) * f32(k_a))
    rh, kh, vh, wh, ah = hd(r), hd(k), hd(v), hd(decay), hd(a)

    def step(S, inp):
        r_t, w_t, kk_t, a_t, v_t, k_t = inp
        sa = jnp.einsum('bhvk,bhk->bhv', S, -kk_t)
        S = S * w_t[:, :, None, :] + sa[..., None] * (kk_t * a_t)[:, :, None, :] + v_t[..., None] * k_t[:, :, None, :]
        y = jnp.einsum('bhvk,bhk->bhv', S, r_t)
        return S, y

    xs = tuple(jnp.moveaxis(t, 1, 0) for t in (rh, wh, kk, ah, vh, kh))
    S0 = jnp.zeros((B, R_HEADS, R_HEAD_DIM, R_HEAD_DIM), jnp.float32)
    _, y = lax.scan(step, S0, xs)
    y = jnp.moveaxis(y, 0, 1)
    m = jnp.mean(y, axis=-1, keepdims=True)
    var = jnp.mean(jnp.square(y - m), axis=-1, keepdims=True)
    y = ((y - m) * lax.rsqrt(var + GN_EPS)).reshape(B, L, R_WIDTH) * f32(gn_g) + f32(gn_b)
    bonus = (jnp.sum(rh * kh * f32(r_k), axis=-1, keepdims=True) * vh).reshape(B, L, R_WIDTH)
    return ((y + bonus) * g).astype(dt)


def token_mixers(h, w_in, b_gate, mu, w0, w_up, a0, a_up, g_up, k_k, k_a, r_k, gn_g, gn_b, w_branch_a, w_branch_b, w_out, topk):
    B, L, _ = h.shape
    u = h @ w_in
    ua, ur, ug = split_cols(u, [A_COLS, R_COLS, GATE_COLS])
    q, k, v, qi, ki, wi = split_cols(ua, [A_Q, A_KV, A_KV, IDX_Q, IDX_DIM, IDX_HEADS])
    o_a = dsa_attention(q.reshape(B, L, A_HEADS, A_HEAD_DIM), k.reshape(B, L, A_KV_HEADS, A_HEAD_DIM),
                        v.reshape(B, L, A_KV_HEADS, A_HEAD_DIM), qi.reshape(B, L, IDX_HEADS, IDX_DIM), ki, wi, topk)
    o_b = rwkv7_time_mix(ur, mu, w0, w_up, a0, a_up, g_up, k_k, k_a, r_k, gn_g, gn_b)
    gates = jax.nn.sigmoid((ug + b_gate).astype(jnp.float32)).astype(h.dtype)
    g_a, g_b = split_cols(gates, [D_MODEL, D_MODEL])
    merged = g_a * (o_a @ w_branch_a) + g_b * (o_b @ w_branch_b)
    return merged @ w_out


def grouped_moe(h, w_router, b_router, w1, w3, w2):
    B, L, D = h.shape
    x = h.reshape(B * L, D)
    T = x.shape[0]
    s = jax.nn.sigmoid((x @ w_router).astype(jnp.float32))
    sel = s + b_router.astype(jnp.float32)
    sg = sel.reshape(T, N_GROUPS, EXPERTS_PER_GROUP)
    group_score = jnp.sum(lax.top_k(sg, TOP_K_EXPERTS)[0], axis=-1)
    g_star = jnp.argmax(group_score, axis=-1)
    in_group = sg[jnp.arange(T), g_star]
    _, j = lax.top_k(in_group, TOP_K_EXPERTS)
    experts = g_star[:, None] * EXPERTS_PER_GROUP + j
    gate = jnp.take_along_axis(s, experts, axis=-1)
    gate = gate / jnp.sum(gate, axis=-1, keepdims=True)
    combine = jnp.sum(jax.nn.one_hot(experts, N_EXPERTS, dtype=jnp.float32) * gate[..., None], axis=1).astype(x.dtype)
    y = jnp.zeros_like(x)
    for e in range(N_EXPERTS):
        he = (jax.nn.silu(x @ w1[e]) * (x @ w3[e])) @ w2[e]
        y = y + combine[:, e:e + 1] * he
    return y.reshape(B, L, D)


def setup_inputs(seed: int = 0) -> dict:
    key = jax.random.key(seed)
    ks = jax.random.split(key, 32)
    nrm = lambda k, shape, s: jax.random.normal(k, shape, jnp.float32) * s
    uni = lambda k, shape, lo, hi: jax.random.uniform(k, shape, jnp.float32, lo, hi)
    return {
        'x': nrm(ks[0], (BATCH, SEQ, D_MODEL), 1.0),
        'meta_tokens': nrm(ks[1], (N_META, D_MODEL), 1.0),
        'ln_in_g': 1.0 + nrm(ks[2], (D_MODEL,), 0.02),
        'ln_in_b': nrm(ks[3], (D_MODEL,), 0.02),
        'w_in': nrm(ks[4], (DEPTH, D_MODEL, IN_COLS), D_MODEL ** -0.5),
        'b_gate': nrm(ks[5], (DEPTH, GATE_COLS), 0.02),
        'rwkv_mu': uni(ks[6], (DEPTH, R_COLS), 0.0, 1.0),
        'rwkv_w0': uni(ks[7], (DEPTH, R_WIDTH), -6.0, 1.0),
        'rwkv_w_up': nrm(ks[8], (DEPTH, W_LORA, R_WIDTH), 0.1),
        'rwkv_a0': nrm(ks[9], (DEPTH, R_WIDTH), 0.5),
        'rwkv_a_up': nrm(ks[10], (DEPTH, A_LORA, R_WIDTH), 0.5 * A_LORA ** -0.5),
        'rwkv_g_up': nrm(ks[11], (DEPTH, G_LORA, R_WIDTH), G_LORA ** -0.5),
        'rwkv_k_k': 0.85 + nrm(ks[12], (DEPTH, R_WIDTH), 0.05),
        'rwkv_k_a': 1.0 + nrm(ks[13], (DEPTH, R_WIDTH), 0.05),
        'rwkv_r_k': nrm(ks[14], (DEPTH, R_HEADS, R_HEAD_DIM), 0.1),
        'rwkv_gn_g': 1.0 + nrm(ks[15], (DEPTH, R_WIDTH), 0.02),
        'rwkv_gn_b': nrm(ks[16], (DEPTH, R_WIDTH), 0.02),
        'w_branch_a': nrm(ks[17], (DEPTH, A_Q, D_MODEL), BETA * A_Q ** -0.5),
        'w_branch_b': nrm(ks[18], (DEPTH, R_WIDTH, D_MODEL), BETA * R_WIDTH ** -0.5),
        'w_out': nrm(ks[19], (DEPTH, D_MODEL, D_MODEL), BETA * D_MODEL ** -0.5),
        'ln1_g': 1.0 + nrm(ks[20], (DEPTH, D_MODEL), 0.02),
        'ln1_b': nrm(ks[21], (DEPTH, D_MODEL), 0.02),
        'ln2_g': 1.0 + nrm(ks[22], (DEPTH, D_MODEL), 0.02),
        'ln2_b': nrm(ks[23], (DEPTH, D_MODEL), 0.02),
        'w_router': nrm(ks[24], (D_MODEL, N_EXPERTS), D_MODEL ** -0.5),
        'b_router': nrm(ks[25], (N_EXPERTS,), 0.01),
        'w_exp1': nrm(ks[26], (DEPTH, N_EXPERTS, D_MODEL, D_EXPERT), D_MODEL ** -0.5),
        'w_exp3': nrm(ks[27], (DEPTH, N_EXPERTS, D_MODEL, D_EXPERT), D_MODEL ** -0.5),
        'w_exp2': nrm(ks[28], (DEPTH, N_EXPERTS, D_EXPERT, D_MODEL), BETA * D_EXPERT ** -0.5),
    }


def reference(x, meta_tokens, ln_in_g, ln_in_b, w_in, b_gate, rwkv_mu, rwkv_w0, rwkv_w_up, rwkv_a0, rwkv_a_up,
              rwkv_g_up, rwkv_k_k, rwkv_k_a, rwkv_r_k, rwkv_gn_g, rwkv_gn_b, w_branch_a, w_branch_b, w_out,
              ln1_g, ln1_b, ln2_g, ln2_b, w_router, b_router, w_exp1, w_exp3, w_exp2):
    B, S, D = x.shape
    topk = min(TOPK_MAX, S // 4)
    meta = jnp.broadcast_to(meta_tokens[None].astype(x.dtype), (B, N_META, D))
    h = layer_norm(jnp.concatenate([meta, x], axis=1), ln_in_g, ln_in_b)
    for l in range(DEPTH):
        mix = token_mixers(h, w_in[l], b_gate[l], rwkv_mu[l], rwkv_w0[l], rwkv_w_up[l], rwkv_a0[l], rwkv_a_up[l],
                           rwkv_g_up[l], rwkv_k_k[l], rwkv_k_a[l], rwkv_r_k[l], rwkv_gn_g[l], rwkv_gn_b[l],
                           w_branch_a[l], w_branch_b[l], w_out[l], topk)
        h = layer_norm(ALPHA * h + mix, ln1_g[l], ln1_b[l])
        ffn = grouped_moe(h, w_router, b_router, w_exp1[l], w_exp3[l], w_exp2[l])
        h = layer_norm(ALPHA * h + ffn, ln2_g[l], ln2_b[l])
    return h[:, N_META:]
```

```python
import numpy as np
from contextlib import ExitStack
import concourse.bass as bass
import concourse.mybir as mybir
from concourse.bass_utils import run_bass_kernel_spmd

F32 = mybir.dt.float32
BF16 = mybir.dt.bfloat16
AF = mybir.ActivationFunctionType
ALU = mybir.AluOpType
AX = mybir.AxisListType

D = 1024
LR = 4112
NT = 34
LP = NT * 128
NOWN = 17
TOWN = NOWN * 128
SEGS = [(s * 512, 512) for s in range(8)] + [(4096, 256)]
ALPHA = 4.0 ** 0.25
LN_EPS = 1e-5
GN_EPS = 64e-5
NEG = -30000.0
NBIS = 20
UO = 4
DBG = {}
CDEC = float(np.exp(-0.5))


class Buf:
    __slots__ = ("w", "r", "name")

    def __init__(self, name=""):
        self.w = None
        self.r = {}
        self.name = name


class Tl:
    def __init__(self, t, name):
        self.t = t
        self.b = Buf(name)

    def __getitem__(self, idx):
        return self.t[idx]


class Sched:
    ENGS = ["pe", "act", "dve", "pool", "sp"]
    QS = ["sp", "act", "pool"]
    NSLOT = 8

    def __init__(self, nc, es):
        self.nc = nc
        self.prog = {e: [] for e in self.ENGS}
        self.cnt = {e: 0 for e in self.ENGS}
        self.known = {e: {} for e in self.ENGS}
        self.sems = {}
        for e in self.ENGS:
            self.sems[("c", e)] = es.enter_context(nc.semaphore("c_" + e))
        self.dcnt = {}
        for q in self.QS:
            self.dcnt[q] = 0
            for s in range(self.NSLOT):
                self.sems[("d", q, s)] = es.enter_context(nc.semaphore(f"d_{q}_{s}"))
        self.nins = 0
        self.sems[("cc",)] = es.enter_context(nc.semaphore("cc_sem"))
        self.ccnt = 0

    def coll(self, fn, reads=(), writes=()):
        reads = [x.b if isinstance(x, Tl) else x for x in reads]
        writes = [x.b if isinstance(x, Tl) else x for x in writes]
        self._wait("pool", self._deps("pool", reads, writes))
        self.ccnt += 1
        sem = self.sems[("cc",)]
        self.prog["pool"].append(lambda eng, fn=fn, sem=sem: fn(eng).then_inc(sem, 1))
        self._mark((("cc",), self.ccnt), reads, writes)

    def _wait(self, e, deps):
        kn = self.known[e]
        for key, val in deps.items():
            if kn.get(key, 0) >= val:
                continue
            kn[key] = val
            sem = self.sems[key]
            self.prog[e].append(lambda eng, sem=sem, val=val: eng.wait_ge(sem, val))

    def _deps(self, e, reads, writes):
        deps = {}

        def add(k, v):
            if e == "pe" and k == ("c", "pe"):
                return
            if deps.get(k, 0) < v:
                deps[k] = v

        for b in reads:
            if b.w is not None:
                add(*b.w)
        for b in writes:
            if b.w is not None:
                add(*b.w)
            for k, v in b.r.items():
                add(k, v)
        return deps

    def _mark(self, tok, reads, writes):
        k, v = tok
        for b in reads:
            if b.r.get(k, 0) < v:
                b.r[k] = v
        for b in writes:
            b.w = tok
            b.r = {}

    def op(self, e, fn, reads=(), writes=(), inc=True):
        reads = [x.b if isinstance(x, Tl) else x for x in reads]
        writes = [x.b if isinstance(x, Tl) else x for x in writes]
        self._wait(e, self._deps(e, reads, writes))
        self.nins += 1
        if inc:
            self.cnt[e] += 1
            tok = (("c", e), self.cnt[e])
            sem = self.sems[("c", e)]
            self.prog[e].append(lambda eng, fn=fn, sem=sem: fn(eng).then_inc(sem, 1))
        else:
            tok = (("c", e), self.cnt[e] + 1)
            self.prog[e].append(lambda eng, fn=fn: fn(eng))
        self._mark(tok, reads, writes)

    def dma(self, q, out, in_, reads=(), writes=(), fn=None):
        reads = [x.b if isinstance(x, Tl) else x for x in reads]
        writes = [x.b if isinstance(x, Tl) else x for x in writes]
        i = self.dcnt[q]
        self.dcnt[q] += 1
        slot = i % self.NSLOT
        key = ("d", q, slot)
        deps = self._deps(q, reads, writes)
        if i >= self.NSLOT:
            prev = 16 * (i // self.NSLOT)
            if deps.get(key, 0) < prev:
                deps[key] = prev
        self._wait(q, deps)
        val = 16 * (i // self.NSLOT + 1)
        sem = self.sems[key]
        self.nins += 1
        if fn is not None:
            self.prog[q].append(lambda eng, fn=fn, sem=sem: fn(eng).then_inc(sem, 16))
        else:
            self.prog[q].append(lambda eng, out=out, in_=in_, sem=sem: eng.dma_start(out=out, in_=in_).then_inc(sem, 16))
        self._mark((key, val), reads, writes)

    def _alldeps(self):
        deps = {}
        for q in self.QS:
            n = self.dcnt[q]
            for s in range(self.NSLOT):
                c = (n - s + self.NSLOT - 1) // self.NSLOT if n > s else 0
                if c > 0:
                    deps[("d", q, s)] = 16 * c
        for e in self.ENGS:
            if self.cnt[e] > 0:
                deps[("c", e)] = self.cnt[e]
        if self.ccnt > 0:
            deps[("cc",)] = self.ccnt
        return deps

    def barrier(self):
        deps = self._alldeps()
        for e in self.ENGS:
            d = {k: v for k, v in deps.items() if k != ("c", e)}
            self._wait(e, d)

    def finish(self):
        self.barrier()

    def emit(self):
        nc = self.nc
        with nc.Block() as block:
            @block.tensor
            def _(eng):
                for f in self.prog["pe"]:
                    f(eng)

            @block.scalar
            def _(eng):
                for f in self.prog["act"]:
                    f(eng)

            @block.vector
            def _(eng):
                for f in self.prog["dve"]:
                    f(eng)

            @block.gpsimd
            def _(eng):
                for f in self.prog["pool"]:
                    f(eng)

            @block.sync
            def _(eng):
                for f in self.prog["sp"]:
                    f(eng)


class KB:
    def __init__(self):
        self.nc = bass.Bass("TRN2", target_bir_lowering=False)
        self.es = ExitStack()
        self.S = Sched(self.nc, self.es)
        self.n = 0
        self.rr = 0
        self.esd = self.es

    def din(self, name, shape, dt=F32):
        return self.nc.dram_tensor(name, list(shape), dt, kind="ExternalInput").ap()

    def dout(self, name, shape, dt=F32):
        return self.nc.dram_tensor(name, list(shape), dt, kind="ExternalOutput").ap()

    def sb(self, name, shape, dt=F32, es=None):
        self.n += 1
        nm = f"{name}_{self.n}"
        return Tl((es or self.esd).enter_context(self.nc.sbuf_tensor(nm, list(shape), dt)), nm)

    def ps(self, name, shape, dt=F32, es=None):
        self.n += 1
        nm = f"{name}_{self.n}"
        return Tl((es or self.esd).enter_context(self.nc.psum_tensor(nm, list(shape), dt)), nm)

    def mm(self, out, lhsT, rhs, start, stop, reads, writes, inc=True, skip=False):
        self.S.op("pe", lambda e: e.matmul(out, lhsT=lhsT, rhs=rhs, start=start, stop=stop, skip_group_check=skip), reads=reads, writes=writes, inc=inc)

    def tr(self, out, in_, ident, reads, writes, inc=True):
        self.S.op("pe", lambda e: e.transpose(out=out, in_=in_, identity=ident), reads=reads, writes=writes, inc=inc)

    def act(self, out, in_, func, reads, writes, bias=None, scale=1.0, accum=None):
        kw = {}
        if bias is not None:
            kw["bias"] = bias
        if accum is not None:
            kw["accum_out"] = accum
        self.S.op("act", lambda e: e.activation(out=out, in_=in_, func=func, scale=scale, **kw), reads=reads, writes=writes)

    def tt(self, out, in0, in1, op, reads, writes, eng="dve"):
        self.S.op(eng, lambda e: e.tensor_tensor(out=out, in0=in0, in1=in1, op=op), reads=reads, writes=writes)

    def ts(self, out, in0, s1, op0, reads, writes, s2=None, op1=None, accum=None, eng="dve"):
        kw = {}
        if op1 is not None:
            kw["op1"] = op1
        if accum is not None:
            kw["accum_out"] = accum
        self.S.op(eng, lambda e: e.tensor_scalar(out=out, in0=in0, scalar1=s1, scalar2=s2, op0=op0, **kw), reads=reads, writes=writes)

    def stt(self, out, in0, scalar, in1, op0, op1, reads, writes):
        self.S.op("dve", lambda e: e.scalar_tensor_tensor(out=out, in0=in0, scalar=scalar, in1=in1, op0=op0, op1=op1), reads=reads, writes=writes)

    def copy(self, out, in_, reads, writes, eng=None):
        if eng is None:
            self.rr += 1
            eng = "act" if self.rr % 2 else "dve"
        if eng == "act":
            self.S.op("act", lambda e: e.activation(out=out, in_=in_, func=AF.Copy), reads=reads, writes=writes)
        else:
            self.S.op(eng, lambda e: e.tensor_copy(out=out, in_=in_), reads=reads, writes=writes)

    def recip(self, out, in_, reads, writes):
        self.S.op("dve", lambda e: e.reciprocal(out=out, in_=in_), reads=reads, writes=writes)

    def memset(self, ap, val, writes, eng="pool"):
        self.S.op(eng, lambda e: e.memset(ap, val), writes=writes)

    def done(self):
        self.S.finish()
        self.S.emit()
        self.es.close()
        return self.nc


def layer_norm_tile(k, x, gb, out, tmp, stat, reads_extra=()):
    nchunk = 2
    st = stat
    for c in range(nchunk):
        k.S.op("dve", lambda e, c=c: e.bn_stats(out=st[:, c, :], in_=x[:, c * 512:(c + 1) * 512]), reads=[x], writes=[st])
    k.S.op("dve", lambda e: e.bn_aggr(out=st[:, 2, 0:2], in_=st[:, 0:2, :]), reads=[st], writes=[st])
    k.ts(st[:, 3, 0:1], st[:, 2, 1:2], LN_EPS, ALU.add, [st], [st])
    k.act(st[:, 3, 1:2], st[:, 3, 0:1], AF.Sqrt, [st], [st])
    k.S.op("dve", lambda e: e.reciprocal(out=st[:, 3, 2:3], in_=st[:, 3, 1:2]), reads=[st], writes=[st])
    k.ts(tmp[:, :], x[:, :], st[:, 2, 0:1], ALU.subtract, [x, st], [tmp], s2=st[:, 3, 2:3], op1=ALU.mult)
    k.tt(tmp[:, :], tmp[:, :], gb[:, 0, :], ALU.mult, [tmp, gb], [tmp], eng="pool")
    k.tt(out[:, :], tmp[:, :], gb[:, 1, :], ALU.add, [tmp, gb], [out])


def build_L0(k=None, io=None):
    alone = k is None
    if alone:
        k = KB()
        io = dict(x=k.din("x", [TOWN, D]), g=k.din("g", [1, D]), b=k.din("b", [1, D]), y=k.dout("y", [TOWN, D]))
    x, g, b, y = io["x"], io["g"], io["b"], io["y"]
    gb = k.sb("gb", [128, 2, D])
    k.S.dma("sp", gb[:, 0, :], g[0:1, :].to_broadcast([128, D]), writes=[gb])
    k.S.dma("sp", gb[:, 1, :], b[0:1, :].to_broadcast([128, D]), writes=[gb])
    xs = [k.sb("x", [128, D]) for _ in range(2)]
    ts_ = [k.sb("t", [128, D]) for _ in range(2)]
    os_ = [k.sb("o", [128, D]) for _ in range(2)]
    sts = [k.sb("st", [128, 4, 6]) for _ in range(2)]
    for i in range(NOWN):
        p = i % 2
        k.S.dma("sp", xs[p][:, :], x[i * 128:(i + 1) * 128, :], writes=[xs[p]])
        layer_norm_tile(k, xs[p], gb, os_[p], ts_[p], sts[p])
        k.S.dma("act", y[i * 128:(i + 1) * 128, :], os_[p][:, :], reads=[os_[p]])
    if alone:
        return k.done()
    k.S.barrier()


C_ID = 0
C_BD = 128
C_MAB = 256
C_MC = 384
C_RST = 448
C_ADM = 960
C_MAB4 = 1344
C_MC4 = 1856
C_ID4 = 2112
C_END = 2368
NPV = 7


def L1_io(k, sfx=""):
    return dict(wA=k.din("wA" + sfx, [D, 480]), wR=k.din("wR" + sfx, [D, 1024]), muR=k.din("muR" + sfx, [128, 8]),
                wO=k.din("wO" + sfx, [D, 776]), pvec=k.din("pvec" + sfx, [128, 2 * NPV]), wup=k.din("wup" + sfx, [64, 256]),
                aup=k.din("aup" + sfx, [64, 256]), gup=k.din("gup" + sfx, [128, 256]))


def build_L1(k=None, io=None, banks=None):
    alone = k is None
    if alone:
        k = KB()
        io = L1_io(k)
        hin_ = k.din("hin", [LP, D])
        obT_ = k.dout("obT", [256, LP])
        io.update(hown=k.din("hown", [TOWN, D]), cst=k.din("cst", [128, C_END]), oa=k.dout("oa", [TOWN, 512]),
                  hin_tile=lambda j: hin_[j * 128:(j + 1) * 128, :],
                  ob_out=lambda ct, j: obT_[ct * 128:(ct + 1) * 128, j * 128:(j + 1) * 128])
    S = k.S
    hown = io["hown"]
    wA_d, wR_d, muR_d, wO_d, pvec_d = io["wA"], io["wR"], io["muR"], io["wO"], io["pvec"]
    wup_d, aup_d, gup_d, cst_d, oa_d = io["wup"], io["aup"], io["gup"], io["cst"], io["oa"]
    hin_tile, ob_out = io["hin_tile"], io["ob_out"]
    hin = None

    cst = k.sb("cst", [128, C_END])
    S.dma("sp", cst[:, :], cst_d[:, :], writes=[cst])
    identb = k.sb("identb", [128, 128], BF16)
    k.copy(identb[:, :], cst[:, C_ID:C_ID + 128], [cst], [identb], eng="dve")
    ident = cst.t[:, C_ID:C_ID + 128]
    bdones = cst.t[:, C_BD:C_BD + 128]
    pvec = k.sb("pvec", [128, 2, NPV + 1])
    S.dma("sp", pvec[:, :, 0:NPV], pvec_d.rearrange("p (c v) -> p c v", c=2), writes=[pvec])
    k.ts(pvec[:, :, NPV:NPV + 1], pvec[:, :, 3:4], -1.0, ALU.mult, [pvec], [pvec], s2=1.0, op1=ALU.add)
    muR = k.sb("muR", [128, 8])
    S.dma("sp", muR[:, :], muR_d[:, :], writes=[muR])
    wup = k.sb("wup", [64, 256]); S.dma("act", wup[:, :], wup_d[:, :], writes=[wup])
    aup = k.sb("aup", [128, 256]); S.dma("act", aup[64:128, :], aup_d[:, :], writes=[aup])
    gup = k.sb("gup", [128, 256]); S.dma("act", gup[:, :], gup_d[:, :], writes=[gup])

    kT = k.sb("kT", [128, 2, LP], BF16)
    kiT = k.sb("kiT", [96, LP], BF16)
    Vaug = k.sb("Vaug", [128, NT, 2, 65], BF16)
    k.memset(Vaug[:, :, :, 64:65], 1.0, [Vaug])

    if banks is None:
        banks = [k.ps(f"bank{i}", [128, 512]) for i in range(8)]

    es1 = ExitStack()
    wA = k.sb("wA", [128, 8, 480], BF16, es1)
    wR = k.sb("wR", [128, 8, 1024], BF16, es1)
    S.dma("pool", wA[:, :, :], wA_d.rearrange("(kt p) c -> p kt c", p=128), writes=[wA])
    for kt in range(8):
        S.dma("pool", wR[:, kt, :], wR_d[kt * 128:(kt + 1) * 128, :], writes=[wR])
    hst = [k.sb("hst", [128, D], F32, es1)] * 2
    hT = k.sb("hT", [128, 8, 512], BF16, es1)
    usb = k.sb("usb", [128, 8, 516], F32, es1)
    k.memset(usb[:, :, UO - 1:UO], 0.0, [usb])
    halo = k.sb("halo", [128, 8, 1], F32, es1)
    def r1(name, shape=(128, 512)):
        return k.sb(name, list(shape), F32, es1)
    tw = r1("tw", (64, 512)); sgx = r1("sgx")
    ld = r1("ld"); Lc = r1("Lc"); av = r1("av"); kk = r1("kk"); sq = r1("sq"); rn = r1("rn"); kp = r1("kp"); bv = r1("bv")
    E = r1("E"); Einv = r1("Einv"); Eprev = av; Eend = ld; tmpa = sq; dtmp = rn
    gam = k.sb("gam", [128, 2, 8], F32, es1)
    RD = BF16 if DBG.get("rbf16", 0) else F32
    AR = k.sb("AR", [128, 2, 8, 128], RD, es1)
    bt = k.sb("bt", [128, 2, 512], RD, es1)
    kt_ = k.sb("kt", [128, 2, 512], RD, es1)
    Kh = k.sb("Kh", [128, 2, 512], RD, es1)
    Bh = k.sb("Bh", [128, 2, 512], RD, es1)
    KhTM = k.sb("KhTM", [64, 8, 256], RD, es1)
    BhTM = k.sb("BhTM", [64, 8, 256], RD, es1)
    Vtm = k.sb("Vtm", [64, 8, 256], RD, es1)
    Vpad = k.sb("Vpad", [64, 4, 128], RD, es1)
    k.memset(Vpad[:, :, :], 0.0, [Vpad])
    Upad = k.sb("Upad", [64, 4, 128], RD, es1)
    k.memset(Upad[:, :, :], 0.0, [Upad])
    Usb = k.sb("Usb", [64, 256], RD, es1)
    Wsb = k.sb("Wsb", [64, 256], RD, es1)
    SA = [k.sb("SA", [64, 4, 128], RD, es1) for _ in range(2)]
    SB = [k.sb("SB", [64, 4, 128], RD, es1) for _ in range(2)]
    X = [k.sb("X", [64, 4, 64], F32, es1) for _ in range(2)]
    XB = [k.sb("XB", [64, 4, 64], RD, es1) for _ in range(2)] if RD != F32 else X
    PQ = [[k.sb("PQ", [64, 2, 4, 64], RD, es1) for _ in range(2)]] * 2
    Hbd = k.sb("Hbd", [128, 2, 128], F32, es1)
    k.memset(Hbd[:, :, :], 0.0, [Hbd])
    if RD != F32:
        Hb = k.sb("Hb", [128, 2, 128], RD, es1)
        k.memset(Hb[:, :, :], 0.0, [Hb])
    else:
        Hb = Hbd
    identR = identb.t[:, :] if DBG.get("rbf16", 0) else ident
    yT = k.sb("yT", [128, 2, 512], F32, es1)
    bon = k.sb("bon", [128, 2, 512], F32, es1)
    gv = k.sb("gv", [128, 2, 512], F32, es1)
    ob = yT
    id64 = cst.t[0:64, C_ID:C_ID + 64]

    tile_ctr = [0]

    def load_hT(src, row0, ntiles, dst, reads_dst=()):
        for t in range(ntiles):
            i = tile_ctr[0]; tile_ctr[0] += 1
            hs = hst[i % 2]
            S.dma("sp" if i % 2 == 0 else "act", hs[:, :], hin_tile(row0 // 128 + t), writes=[hs])
            for half in range(2):
                bk = banks[half]
                for q in range(4):
                    kt = half * 4 + q
                    k.tr(bk[:, q * 128:(q + 1) * 128], hs[:, kt * 128:(kt + 1) * 128], ident, [hs, cst], [bk], inc=(q == 3))
                k.copy(dst[:, half * 4:half * 4 + 4, t * 128:(t + 1) * 128], bk.t[:, :].rearrange("p (q c) -> p q c", q=4), [bk], [dst])

    def proj_cm(dst_ap, dst_tl, w, c0, M, N, bank, extra_reads=()):
        for kt in range(8):
            k.mm(bank[0:M, 0:N], w[:, kt, c0:c0 + M], hT[:, kt, 0:N], kt == 0, kt == 7, [w, hT], [bank], inc=(kt == 7))
        k.copy(dst_ap, bank[0:M, 0:N], [bank], [dst_tl])

    for (t0, N) in SEGS:
        ntl = N // 128
        nch = N // 64
        load_hT(hin, t0, ntl, hT)
        for g in range(2):
            proj_cm(kT[:, g, t0:t0 + N], kT, wA, g * 128, 128, N, banks[2 + g])
        proj_cm(kiT[:, t0:t0 + N], kiT, wA, 256, 96, N, banks[4])
        for t in range(ntl):
            bk = banks[5 + t % 2]
            for kt in range(8):
                k.mm(bk[:, 0:128], hT[:, kt, t * 128:(t + 1) * 128], wA[:, kt, 352:480], kt == 0, kt == 7, [hT, wA], [bk], inc=(kt == 7))
            k.copy(Vaug[:, t0 // 128 + t, :, 0:64], bk.t[:, 0:128].rearrange("p (g d) -> p g d", g=2), [bk], [Vaug])
        if not DBG.get("rwkv", 1):
            continue
        for m in range(8):
            proj_cm(usb[:, m, UO:UO + N], usb, wR, m * 128, 128, N, banks[2 + m % 4])
        k.copy(halo[:, :, :], usb[:, :, UO + N - 1:UO + N], [usb], [halo], eng="pool")
        for m in range(8):
            k.tt(dtmp[:, 0:N], usb[:, m, UO - 1:UO - 1 + N], usb[:, m, UO:UO + N], ALU.subtract, [usb], [dtmp])
            k.stt(usb[:, m, UO:UO + N], dtmp[:, 0:N], muR[:, m:m + 1], usb[:, m, UO:UO + N], ALU.mult, ALU.add, [dtmp, muR, usb], [usb])
        k.copy(usb[:, :, UO - 1:UO], halo[:, :, :], [halo], [usb], eng="pool")
        if DBG.get("rstage", 9) < 1:
            continue
        def U(m, lo=0, hi=128):
            return usb[lo:hi, m, UO:UO + N]
        k.act(tw[:, 0:N], U(6, 0, 64), AF.Tanh, [usb], [tw])
        k.act(sgx[:, 0:N], U(7), AF.Sigmoid, [usb], [sgx])
        if DBG.get("rstage", 9) < 2:
            continue
        for ct in range(2):
            pv = lambda j: pvec[:, ct, j:j + 1]
            cs = slice(ct * 128, (ct + 1) * 128)
            b0, b1, b2 = banks[2], banks[3], banks[4]
            k.mm(b0[:, 0:N], wup[:, cs], tw[:, 0:N], True, True, [wup, tw], [b0])
            k.act(ld[:, 0:N], b0[:, 0:N], AF.Sigmoid, [b0, pvec], [ld], bias=pv(0))
            S.op("dve", lambda e, o=Lc[:, 0:N], d0=cst[:, C_RST:C_RST + N], d1=ld[:, 0:N]: e.tensor_tensor_scan(out=o, data0=d0, data1=d1, initial=0.0, op0=ALU.mult, op1=ALU.add), reads=[cst, ld], writes=[Lc])
            k.mm(b1[:, 0:N], aup[64:128, cs], U(6, 64, 128), True, True, [aup, usb], [b1])
            k.act(av[:, 0:N], b1[:, 0:N], AF.Sigmoid, [b1, pvec], [av], bias=pv(1))
            k.mm(b2[:, 0:N], gup[:, cs], sgx[:, 0:N], True, True, [gup, sgx], [b2])
            k.copy(gv[:, ct, 0:N], b2[:, 0:N], [b2], [gv], eng="dve")
            k.ts(kk[:, 0:N], U(2 + ct), pv(2), ALU.mult, [usb, pvec], [kk])
            k.act(sq[:, 0:N], kk[:, 0:N], AF.Square, [kk], [sq])
            k.mm(b0[:, 0:N], bdones, sq[:, 0:N], True, True, [cst, sq], [b0])
            k.ts(rn[:, 0:N], b0[:, 0:N], 1e-24, ALU.max, [b0], [rn])
            k.act(rn[:, 0:N], rn[:, 0:N], AF.Sqrt, [rn], [rn])
            k.recip(rn[:, 0:N], rn[:, 0:N], [rn], [rn])
            k.tt(kk[:, 0:N], kk[:, 0:N], rn[:, 0:N], ALU.mult, [kk, rn], [kk])
            k.ts(kp[:, 0:N], av[:, 0:N], pv(3), ALU.mult, [av, pvec], [kp], s2=pv(NPV), op1=ALU.add)
            k.tt(kp[:, 0:N], kp[:, 0:N], U(2 + ct), ALU.mult, [kp, usb], [kp])
            k.tt(bv[:, 0:N], kk[:, 0:N], av[:, 0:N], ALU.mult, [kk, av], [bv], eng="pool")
            k.stt(sq[:, 0:N], U(ct), pv(4), kp[:, 0:N], ALU.mult, ALU.mult, [usb, pvec, kp], [sq])
            k.mm(b1[:, 0:N], bdones, sq[:, 0:N], True, True, [cst, sq], [b1])
            k.tt(bon[:, ct, 0:N], b1[:, 0:N], U(4 + ct), ALU.mult, [b1, usb], [bon])
            k.act(E[:, 0:N], Lc[:, 0:N], AF.Exp, [Lc], [E], scale=-CDEC)
            k.act(Einv[:, 0:N], Lc[:, 0:N], AF.Exp, [Lc], [Einv], scale=CDEC)
            k.tt(tmpa[:, 0:N], Lc[:, 0:N], ld[:, 0:N], ALU.subtract, [Lc, ld], [tmpa], eng="pool")
            k.act(Eprev[:, 0:N], tmpa[:, 0:N], AF.Exp, [tmpa], [Eprev], scale=-CDEC)
            L3 = Lc.t[:, 0:N].rearrange("p (c t) -> p c t", t=64)
            k.tt(rn[:, 0:N].rearrange("p (c t) -> p c t", t=64), L3[:, :, 63:64].to_broadcast([128, nch, 64]), L3, ALU.subtract, [Lc], [rn])
            k.act(Eend[:, 0:N], rn[:, 0:N], AF.Exp, [rn], [Eend], scale=-CDEC)
            k.copy(gam[:, ct, 0:nch], E.t[:, 0:N].rearrange("p (c t) -> p c t", t=64)[:, :, 63], [E], [gam], eng="pool")
            ARv = AR.t[:, ct, 0:nch, :]
            k.stt(ARv[:, :, 0:64], kk[:, 0:N].rearrange("p (c t) -> p c t", t=64), -1.0, Eprev[:, 0:N].rearrange("p (c t) -> p c t", t=64), ALU.mult, ALU.mult, [kk, Eprev], [AR])
            k.tt(ARv[:, :, 64:128], U(ct).rearrange("p (c t) -> p c t", t=64), E[:, 0:N].rearrange("p (c t) -> p c t", t=64), ALU.mult, [usb, E], [AR])
            k.tt(bt[:, ct, 0:N], bv[:, 0:N], Einv[:, 0:N], ALU.mult, [bv, Einv], [bt])
            k.tt(kt_[:, ct, 0:N], kp[:, 0:N], Einv[:, 0:N], ALU.mult, [kp, Einv], [kt_], eng="pool")
            k.tt(Kh[:, ct, 0:N], kp[:, 0:N], Eend[:, 0:N], ALU.mult, [kp, Eend], [Kh])
            k.tt(Bh[:, ct, 0:N], bv[:, 0:N], Eend[:, 0:N], ALU.mult, [bv, Eend], [Bh], eng="pool")
        if DBG.get("rstage", 9) < 3:
            continue
        tmB = [Buf(f"tm{c_}") for c_ in range(8)]

        def st3_gen(c):
            bk = banks[1]
            srcs = [(Kh, 0), (Kh, 1), (Bh, 0), (Bh, 1)]
            for q, (src, ct) in enumerate(srcs):
                k.mm(bk[0:64, q * 128:(q + 1) * 128], src[:, ct, c * 64:(c + 1) * 64], identR, True, True, [src, cst, identb], [bk], inc=(q == 3))
            yield
            k.copy(KhTM[:, c, :], bk[0:64, 0:256], [bk], [tmB[c]], eng="dve")
            k.copy(BhTM[:, c, :], bk[0:64, 256:512], [bk], [tmB[c]], eng="dve")
            for ct in range(2):
                k.mm(bk[0:64, ct * 128:(ct + 1) * 128], usb[:, 4 + ct, UO + c * 64:UO + (c + 1) * 64], ident, True, True, [usb, cst], [bk], inc=(ct == 1))
            yield
            k.copy(Vtm[:, c, :], bk[0:64, 0:256], [bk], [tmB[c]], eng="dve")

        mAB2 = cst.t[0:64, C_MAB4:C_MAB4 + 256].rearrange("p (h c) -> p h c", h=2)
        mC2 = cst.t[0:64, C_MC4:C_MC4 + 128].rearrange("p (h c) -> p h c", h=2)
        id4 = cst.t[0:64, C_ID4:C_ID4 + 256].rearrange("p (h c) -> p h c", h=4)

        def inv_gen(c):
            cp = c % 2
            sa, sbb, x, xb = SA[cp], SB[cp], X[cp], XB[cp]
            cc = slice(c * 64, (c + 1) * 64)
            for h in range(4):
                ct, hp = h // 2, h % 2
                ps_ = slice(hp * 64, hp * 64 + 64)
                k.mm(banks[4 + hp][0:64, ct * 128:(ct + 1) * 128], bt[ps_, ct, cc], AR[ps_, ct, c, :], True, True, [bt, AR], [banks[4 + hp]])
                k.mm(banks[6 + hp][0:64, ct * 128:(ct + 1) * 128], kt_[ps_, ct, cc], AR[ps_, ct, c, :], True, True, [kt_, AR], [banks[6 + hp]])
            yield
            for hp in range(2):
                sav = sa.t[:, :, :].rearrange("p (a b) c -> p a b c", b=2)[:, :, hp, :]
                sbv = sbb.t[:, :, :].rearrange("p (a b) c -> p a b c", b=2)[:, :, hp, :]
                k.tt(sav, banks[4 + hp].t[0:64, 0:256].rearrange("p (a c) -> p a c", a=2), mAB2, ALU.mult, [banks[4 + hp], cst], [sa])
                k.tt(sbv, banks[6 + hp].t[0:64, 0:256].rearrange("p (a c) -> p a c", a=2), mAB2, ALU.mult, [banks[6 + hp], cst], [sbb])
            for h in range(4):
                ct, hp = h // 2, h % 2
                ps_ = slice(hp * 64, hp * 64 + 64)
                k.mm(banks[4 + hp][0:64, ct * 64:(ct + 1) * 64], AR[ps_, ct, c, 0:64], bt[ps_, ct, cc], True, True, [bt, AR], [banks[4 + hp]])
            yield
            pq0 = PQ[0][0]
            k.copy(pq0[:, 0, :, :], sa[:, :, 0:64], [sa], [pq0], eng="dve")
            for hp in range(2):
                k.tt(pq0.t[:, 1, :, :].rearrange("p (a b) c -> p a b c", b=2)[:, :, hp, :], banks[4 + hp].t[0:64, 0:128].rearrange("p (a c) -> p a c", a=2), mC2, ALU.mult, [banks[4 + hp], cst], [pq0])
            k.tt(x[:, :, :], sa[:, :, 0:64], id4, ALU.add, [sa, cst], [x])
            (k.copy(xb[:, :, :], x[:, :, :], [x], [xb], eng=DBG.get("sheng", "dve")) if RD != F32 else None)
            cur = pq0
            bP, bX = banks[6], banks[7]
            for st in range(0, 6):
                nxt = PQ[0][(st + 1) % 2]
                needP = st < 4
                needQ = st < 5
                needX = st >= 1
                last = None
                for h in range(4):
                    if needP:
                        k.mm(bP[0:64, h * 64:(h + 1) * 64], cur[:, 1, h, :], cur[:, 0, h, :], True, True, [cur], [bP], inc=False)
                    if needQ:
                        k.mm(bP[0:64, 256 + h * 64:256 + (h + 1) * 64], cur[:, 0, h, :], cur[:, 1, h, :], True, True, [cur], [bP], inc=(h == 3))
                if needX:
                    for h in range(4):
                        k.mm(bX[0:64, h * 64:(h + 1) * 64], cur[:, 1, h, :], xb[:, h, :], True, True, [cur, xb], [bX], inc=(h == 3))
                yield
                if needP:
                    k.copy(nxt[:, :, :, :], bP.t[0:64, :].rearrange("p (a h c) -> p a h c", a=2, h=4), [bP], [nxt], eng="dve")
                elif needQ:
                    k.copy(nxt[:, 1, :, :], bP.t[0:64, 256:512].rearrange("p (h c) -> p h c", h=4), [bP], [nxt], eng="dve")
                if needX:
                    k.tt(x[:, :, :], bX.t[0:64, 0:256].rearrange("p (h c) -> p h c", h=4), x[:, :, :], ALU.add, [bX, x], [x])
                    (k.copy(xb[:, :, :], x[:, :, :], [x], [xb], eng=DBG.get("sheng", "dve")) if RD != F32 else None)
                cur = nxt
                if st < 5:
                    yield

        def rec_gen(c):
            cp = c % 2
            sa, sbb, x, xb = SA[cp], SB[cp], X[cp], XB[cp]
            cc = slice(c * 64, (c + 1) * 64)
            for hp in range(2):
                k.copy(Vpad.t[:, :, :].rearrange("p (a b) d -> p a b d", b=2)[:, :, hp, hp * 64:(hp + 1) * 64], Vtm.t[:, c, :].rearrange("p (a b d) -> p a b d", a=2, b=2)[:, :, hp, :], [tmB[c]], [Vpad], eng="dve")
            bW, bU, bY, bH = banks[0], banks[0], banks[2], banks[3]
            for h in range(4):
                ct, hp = h // 2, h % 2
                k.mm(bW[0:64, h * 64:(h + 1) * 64], AR[:, ct, c, 0:64], Hb[:, ct, hp * 64:(hp + 1) * 64], True, False, [AR, Hb], [bW], inc=False)
                k.mm(bW[0:64, h * 64:(h + 1) * 64], sbb[:, h, 0:64], Vtm[:, c, h * 64:(h + 1) * 64], False, True, [sbb, tmB[c]], [bW], inc=(h == 3))
            yield
            k.copy(Wsb[:, :], bW[0:64, 0:256], [bW], [Wsb], eng="dve")
            for h in range(4):
                k.mm(bU[0:64, 256 + h * 64:256 + (h + 1) * 64], xb[:, h, :], Wsb[:, h * 64:(h + 1) * 64], True, True, [xb, Wsb], [bU], inc=(h == 3))
            yield
            k.copy(Usb[:, :], bU[0:64, 256:512], [bU], [Usb], eng="dve")
            for hp in range(2):
                k.copy(Upad.t[:, :, :].rearrange("p (a b) d -> p a b d", b=2)[:, :, hp, hp * 64:(hp + 1) * 64], bU.t[0:64, 256:512].rearrange("p (a b d) -> p a b d", a=2, b=2)[:, :, hp, :], [bU], [Upad], eng="dve")
            for ct in range(2):
                o = bY[:, ct * 64:(ct + 1) * 64]
                k.mm(o, Hb[:, ct, :], AR[:, ct, c, 64:128], True, False, [Hb, AR], [bY], inc=False)
                for hp in range(2):
                    h = 2 * ct + hp
                    k.mm(o, Upad[:, h, :], sa[:, h, 64:128], False, False, [Upad, sa], [bY], inc=False)
                    k.mm(o, Vpad[:, h, :], sbb[:, h, 64:128], False, hp == 1, [Vpad, sbb], [bY], inc=(hp == 1 and ct == 1))
            for ct in range(2):
                o = bH[:, ct * 128:(ct + 1) * 128]
                k.mm(o, KhTM[:, c, ct * 128:(ct + 1) * 128], Vtm[:, c, ct * 128:(ct + 1) * 128], True, False, [tmB[c]], [bH], inc=False)
                k.mm(o, BhTM[:, c, ct * 128:(ct + 1) * 128], Usb[:, ct * 128:(ct + 1) * 128], False, True, [tmB[c], Usb], [bH], inc=(ct == 1))
            yield
            k.copy(yT[:, :, cc], bY.t[:, 0:128].rearrange("p (a t) -> p a t", a=2), [bY], [yT], eng="dve")
            for ct in range(2):
                for hp in range(2):
                    ps_ = slice(hp * 64, hp * 64 + 64)
                    k.stt(Hbd[ps_, ct, ps_], Hbd[ps_, ct, ps_], gam[ps_, ct, c:c + 1], bH[ps_, ct * 128 + hp * 64:ct * 128 + hp * 64 + 64], ALU.mult, ALU.add, [Hbd, gam, bH], [Hbd])
            (k.copy(Hb[:, :, :], Hbd[:, :, :], [Hbd], [Hb], eng=DBG.get("sheng", "dve")) if RD != F32 else None)

        def drive(gens):
            gens = [g for g in gens if g is not None]
            while gens:
                for g in list(gens):
                    try:
                        next(g)
                    except StopIteration:
                        gens.remove(g)

        drive([st3_gen(0)])
        drive([inv_gen(0), st3_gen(1)])
        for c in range(nch):
            drive([rec_gen(c), inv_gen(c + 1) if c + 1 < nch else None, st3_gen(c + 2) if c + 2 < nch else None])
        if DBG.get("rstage", 9) < 4:
            continue
        for ct in range(2):
            pv = lambda j: pvec[:, ct, j:j + 1]
            b0, b1 = banks[3], banks[4]
            k.mm(b0[:, 0:N], bdones, yT[:, ct, 0:N], True, True, [cst, yT], [b0])
            k.act(sq[:, 0:N], yT[:, ct, 0:N], AF.Square, [yT], [sq])
            k.mm(b1[:, 0:N], bdones, sq[:, 0:N], True, True, [cst, sq], [b1])
            k.ts(kk[:, 0:N], b0[:, 0:N], 1.0 / 64, ALU.mult, [b0], [kk])
            k.tt(rn[:, 0:N], kk[:, 0:N], kk[:, 0:N], ALU.mult, [kk], [rn])
            k.stt(rn[:, 0:N], b1[:, 0:N], 1.0 / 64, rn[:, 0:N], ALU.mult, ALU.subtract, [b1, rn], [rn])
            k.ts(rn[:, 0:N], rn[:, 0:N], GN_EPS, ALU.add, [rn], [rn])
            k.act(rn[:, 0:N], rn[:, 0:N], AF.Sqrt, [rn], [rn])
            k.recip(rn[:, 0:N], rn[:, 0:N], [rn], [rn])
            k.tt(kk[:, 0:N], yT[:, ct, 0:N], kk[:, 0:N], ALU.subtract, [yT, kk], [kk])
            k.tt(kk[:, 0:N], kk[:, 0:N], rn[:, 0:N], ALU.mult, [kk, rn], [kk])
            k.ts(kk[:, 0:N], kk[:, 0:N], pv(5), ALU.mult, [kk, pvec], [kk], s2=pv(6), op1=ALU.add)
            k.tt(kk[:, 0:N], kk[:, 0:N], bon[:, ct, 0:N], ALU.add, [kk, bon], [kk], eng="pool")
            k.tt(ob[:, ct, 0:N], kk[:, 0:N], gv[:, ct, 0:N], ALU.mult, [kk, gv], [ob])
            for t in range(ntl):
                S.dma("sp" if t % 2 == 0 else "act", ob_out(ct, t0 // 128 + t), ob[:, ct, t * 128:(t + 1) * 128], reads=[ob])
    S.barrier()
    es1.close()
    return k, dict(hown=hown, wO_d=wO_d, oa_d=oa_d, cst=cst, identb=identb, ident=ident, kT=kT, kiT=kiT, Vaug=Vaug, banks=banks)


def build_L1_full(k=None, io=None, banks=None):
    alone = k is None
    k, c = build_L1(k, io, banks)
    S = k.S
    hown, wO_d, oa_d = c["hown"], c["wO_d"], c["oa_d"]
    cst, identb, ident = c["cst"], c["identb"], c["ident"]
    kT, kiT, Vaug, banks = c["kT"], c["kiT"], c["Vaug"], c["banks"]
    es2 = ExitStack()
    qT = k.sb("qT", [128, 4, TOWN], BF16, es2)
    qiT = k.sb("qiT", [96, 3, TOWN], BF16, es2)
    wi = k.sb("wi", [128, NOWN, 8], F32, es2)
    wO = k.sb("wO", [128, 8, 776], BF16, es2)
    S.dma("pool", wO[:, :, :], wO_d.rearrange("(kt p) c -> p kt c", p=128), writes=[wO])
    hst = [k.sb("hst2", [128, D], F32, es2)] * 2
    hT = k.sb("hT2", [128, 8, 512], BF16, es2)
    Irows = [k.sb("Irow", [128, LP], F32, es2) for _ in range(2)]
    junk = k.sb("junk", [128, LP], mybir.dt.uint8, es2)
    mbTs = [k.sb("mbT", [128, NT, 256], BF16, es2) for _ in range(2)]
    accS = k.sb("accS", [128, 8, 130], F32, es2)
    loall = k.sb("loall", [128, NOWN], F32, es2)
    Rh = [k.sb("Rh", [128, 512], BF16, es2) for _ in range(8)]
    PTs = [k.sb("PT", [128, 512], BF16, es2) for _ in range(3)]
    Dhs = [k.sb("Dh", [128, 8, 128], BF16, es2) for _ in range(2)]
    m01 = [k.sb("m01", [128, 512], F32, es2) for _ in range(2)]
    oasb = k.sb("oasb", [128, 2, 512], F32, es2)
    st = k.sb("stb", [128, 8], F32, es2)
    Hs = k.sb("Hs", [128, NBIS], F32, es2)
    p2 = k.sb("p2", [128, NBIS], F32, es2)
    rec = k.sb("rec", [128, 8], F32, es2)
    for j in range(NBIS):
        k.memset(p2[:, j:j + 1], float(2.0 ** -(j + 1)), [p2])
    adm = cst.t[:, C_ADM:C_ADM + 384]

    tc = 0
    for g0 in range(0, NOWN, 4):
        nt = min(4, NOWN - g0)
        N = nt * 128
        for t in range(nt):
            hs = hst[tc % 2]; tc += 1
            S.dma("sp" if tc % 2 == 0 else "act", hs[:, :], hown[(g0 + t) * 128:(g0 + t + 1) * 128, :], writes=[hs])
            for half in range(2):
                bk = banks[half]
                for q in range(4):
                    kt = half * 4 + q
                    k.tr(bk[:, q * 128:(q + 1) * 128], hs[:, kt * 128:(kt + 1) * 128], ident, [hs, cst], [bk], inc=(q == 3))
                k.copy(hT[:, half * 4:half * 4 + 4, t * 128:(t + 1) * 128], bk.t[:, :].rearrange("p (q c) -> p q c", q=4), [bk], [hT])
        gc = slice(g0 * 128, g0 * 128 + N)
        for m in range(4):
            bk = banks[2 + m % 4]
            for kt in range(8):
                k.mm(bk[:, 0:N], wO[:, kt, m * 128:(m + 1) * 128], hT[:, kt, 0:N], kt == 0, kt == 7, [wO, hT], [bk], inc=(kt == 7))
            k.copy(qT[:, m, gc], bk[:, 0:N], [bk], [qT])
        for m in range(3):
            M = 96 if m < 2 else 64
            bk = banks[2 + m]
            for kt in range(8):
                k.mm(bk[0:M, 0:N], wO[:, kt, 512 + m * 96:512 + m * 96 + M], hT[:, kt, 0:N], kt == 0, kt == 7, [wO, hT], [bk], inc=(kt == 7))
            k.copy(qiT[0:M, m, gc], bk[0:M, 0:N], [bk], [qiT])
        for t in range(nt):
            bk = banks[6 + t % 2]
            for kt in range(8):
                k.mm(bk[:, 0:8], hT[:, kt, t * 128:(t + 1) * 128], wO[:, kt, 768:776], kt == 0, kt == 7, [hT, wO], [bk], inc=(kt == 7))
            k.copy(wi[:, g0 + t, :], bk[:, 0:8], [bk], [wi])

    def indexer(i):
        Ir, Dh_ = Irows[i % 2], Dhs[i % 2]
        nkb = min(2 * i + 3, NT)
        Si = nkb * 128
        for h in range(8):
            k.ts(Dh_[:, h, :], identb[:, :], wi[:, i, h:h + 1], ALU.mult, [identb, wi], [Dh_], eng="pool")
        for s0 in range(0, Si, 512):
            n = min(512, Si - s0)
            for h in range(8):
                bk = banks[5 + h % 2]
                pb = 32 * (h % 3)
                k.mm(bk[:, 0:n], qiT[pb:pb + 32, h // 3, i * 128:(i + 1) * 128], kiT[pb:pb + 32, s0:s0 + n], True, True, [qiT, kiT], [bk])
                k.act(Rh[h][:, 0:n], bk[:, 0:n], AF.Relu, [bk], [Rh[h]])
            bI = banks[7]
            for h in range(8):
                k.mm(bI[:, 0:n], Dh_[:, h, :], Rh[h][:, 0:n], h == 0, h == 7, [Dh_, Rh[h]], [bI], inc=(h == 7))
            k.copy(Ir[:, s0:s0 + n], bI[:, 0:n], [bI], [Ir], eng="act")

    def bisect(i):
        Ir = Irows[i % 2]
        nkb = min(2 * i + 3, NT)
        Si = nkb * 128
        S.op("dve", lambda e, Si=Si, Ir=Ir: e.tensor_reduce(out=st[:, 0:1], in_=Ir[:, 0:Si], axis=AX.X, op=ALU.max, apply_absolute_value=True), reads=[Ir], writes=[st])
        a0 = 2 * i * 128
        k.tt(Ir[:, a0:Si], Ir[:, a0:Si], adm[:, 0:Si - a0], ALU.add, [Ir, cst], [Ir])
        k.ts(st[:, 1:2], st[:, 0:1], -1.0001, ALU.mult, [st], [st], s2=-1e-6, op1=ALU.add)
        k.ts(st[:, 2:3], st[:, 0:1], 2.0002, ALU.mult, [st], [st], s2=2e-6, op1=ALU.add)
        k.ts(Hs[:, :], p2[:, :], st[:, 2:3], ALU.mult, [p2, st], [Hs])
        nb = DBG.get("nbis", NBIS)
        k.tt(st[:, 3:4], st[:, 1:2], Hs[:, 0:1], ALU.add, [st, Hs], [st])
        for it in range(nb):
            k.ts(junk[:, 0:Si], Ir[:, 0:Si], st[:, 3:4], ALU.is_ge, [Ir, st], [junk, st], op1=ALU.add, accum=st[:, 4:5])
            k.stt(st[:, 5:6], st[:, 4:5], 255.5, Hs[:, it:it + 1], ALU.is_ge, ALU.mult, [st, Hs], [st])
            sub = Hs[:, it + 1:it + 2] if it + 1 < nb else Hs[:, it:it + 1]
            k.stt(st[:, 3:4], st[:, 5:6], st[:, 3:4], sub, ALU.add, ALU.subtract, [st, Hs], [st])
        k.copy(loall[:, i:i + 1], st[:, 3:4] if nb > 0 else st[:, 1:2], [st], [loall], eng="dve")

    def masks(i, p, nkbG, mbT):
        Ir = Irows[i % 2]
        nkb = min(2 * i + 3, NT)
        Si = nkb * 128
        for ci, s0 in enumerate(range(0, Si, 512)):
            n = min(512, Si - s0)
            nq = n // 128
            mt = m01[ci % 2]
            k.ts(mt[:, 0:n], Ir[:, s0:s0 + n], loall[:, i:i + 1], ALU.is_ge, [Ir, loall], [mt])
            bk = banks[5 + ci % 2]
            for q in range(nq):
                k.tr(bk[:, q * 128:(q + 1) * 128], mt[:, q * 128:(q + 1) * 128], ident, [mt, cst], [bk], inc=(q == nq - 1))
            kb0 = s0 // 128
            k.ts(mbT[:, kb0:kb0 + nq, p * 128:(p + 1) * 128], bk.t[:, 0:n].rearrange("p (q c) -> p q c", q=nq), -1.0, ALU.add, [bk], [mbT], s2=-NEG, op1=ALU.mult)
        if nkb < nkbG:
            k.memset(mbT[:, nkb:nkbG, p * 128:(p + 1) * 128], NEG, [mbT])

    G = 2
    groups = [list(range(g0, min(g0 + G, NOWN))) for g0 in range(0, NOWN if DBG.get("attn", 1) else 0, G)]

    def gk(tiles):
        return min(2 * tiles[-1] + 3, NT)

    def heads(gi, tiles, hs):
        mbT = mbTs[gi % 2]
        nt = len(tiles)
        NQ = nt * 128
        nkbG = gk(tiles)
        gc = slice(tiles[0] * 128, tiles[0] * 128 + NQ)
        for h in hs:
            g, hp, hp2 = h // 4, h % 2, h // 2
            ps_ = slice(hp * 64, hp * 64 + 64)
            acc = banks[3 + h % 2]

            def qk(kb):
                bq = banks[kb % 3]
                k.mm(bq[:, 0:NQ], kT[ps_, g, kb * 128:(kb + 1) * 128], qT[ps_, hp2, gc], True, False, [kT, qT], [bq], inc=False)
                k.mm(bq[:, 0:NQ], identb[:, :], mbT[:, kb, 0:NQ], False, True, [identb, mbT], [bq])

            qk(0)
            for kb in range(nkbG):
                if kb + 1 < nkbG:
                    qk(kb + 1)
                bq = banks[kb % 3]
                PT = PTs[kb % 3]
                k.act(PT[:, 0:NQ], bq[:, 0:NQ], AF.Exp, [bq], [PT], scale=0.125)
                for p in range(nt):
                    k.mm(acc[:, p * 65:(p + 1) * 65], PT[:, p * 128:(p + 1) * 128], Vaug[:, kb, g, :], (kb == 0 and p == 0), kb == nkbG - 1, [PT, Vaug], [acc], inc=(p == nt - 1), skip=True)
            k.copy(accS[:, h, 0:nt * 65], acc[:, 0:nt * 65], [acc], [accS], eng="act")

    def finish(tiles):
        nt = len(tiles)
        a4 = accS.t[:, :, 0:nt * 65].rearrange("p h (t c) -> p h t c", c=65)
        for p in range(nt):
            S.op("dve", lambda e, o=rec[:, 0:8], i_=a4[:, :, p, 64]: e.reciprocal(out=o, in_=i_), reads=[accS], writes=[rec])
            k.tt(oasb.t[:, p, :].rearrange("p (h d) -> p h d", h=8), a4[:, :, p, 0:64], rec[:, 0:8].unsqueeze(2).to_broadcast([128, 8, 64]), ALU.mult, [accS, rec], [oasb])
            S.dma("sp", oa_d[tiles[p] * 128:(tiles[p] + 1) * 128, :], oasb[:, p, :], reads=[oasb])

    if groups:
        t0_ = groups[0]
        indexer(t0_[0])
        for n_, i in enumerate(t0_):
            bisect(i)
            if n_ + 1 < len(t0_):
                indexer(t0_[n_ + 1])
        for p, i in enumerate(t0_):
            masks(i, p, gk(t0_), mbTs[0])
    for gi, tiles in enumerate(groups):
        nxt = groups[gi + 1] if gi + 1 < len(groups) else []
        if nxt:
            indexer(nxt[0])
        heads(gi, tiles, range(0, min(4, DBG.get("nheads", 8))))
        if nxt:
            bisect(nxt[0])
            if len(nxt) > 1:
                indexer(nxt[1])
        heads(gi, tiles, range(4, DBG.get("nheads", 8)))
        if len(nxt) > 1:
            bisect(nxt[1])
        finish(tiles)
        for p, i in enumerate(nxt):
            masks(i, p, gk(nxt), mbTs[(gi + 1) % 2])
    S.barrier()
    es2.close()
    if alone:
        return k.done()


def make_cst(half):
    c = np.zeros((128, C_END), np.float32)
    c[:, C_ID:C_ID + 128] = np.eye(128, dtype=np.float32)
    bd = np.zeros((128, 128), np.float32)
    bd[:64, :64] = 1.0
    bd[64:, 64:] = 1.0
    c[:, C_BD:C_BD + 128] = bd
    i = np.arange(64)[:, None]
    t = np.arange(64)[None, :]
    c[:64, C_MAB:C_MAB + 64] = (i < t)
    c[:64, C_MAB + 64:C_MAB + 128] = (i <= t)
    c[:64, C_MC:C_MC + 64] = (t < i)
    rst = np.ones(512, np.float32)
    rst[::64] = 0.0
    c[:, C_RST:C_RST + 512] = rst[None, :]
    a = np.arange(128)
    lim = np.where(a < 16, 16, np.where(a < 80, 80, 144)) + 128 * half
    r = np.arange(384)[None, :]
    c[:, C_ADM:C_ADM + 384] = np.where(r < lim[:, None], 0.0, -1e30)
    for h in range(4):
        c[:64, C_MAB4 + h * 128:C_MAB4 + (h + 1) * 128] = c[:64, C_MAB:C_MAB + 128]
        c[:64, C_MC4 + h * 64:C_MC4 + (h + 1) * 64] = c[:64, C_MC:C_MC + 64]
        c[:64, C_ID4 + h * 64:C_ID4 + (h + 1) * 64] = np.eye(64, dtype=np.float32)
    return c


def prep_L1(l, inp, half):
    w = inp["w_in"][l]
    A0, R0 = 0, 1064
    kc = [w[:, 512 + g * 64:512 + (g + 1) * 64] for g in range(2)]
    ki = w[:, 1024:1056]
    wA = np.concatenate([kc[0], kc[0], kc[1], kc[1], ki, ki, ki, w[:, 640:768]], axis=1)
    own = half * 256
    def rc(off, ct):
        return slice(R0 + off + own + ct * 128, R0 + off + own + (ct + 1) * 128)
    cols = [rc(0, 0), rc(0, 1), rc(512, 0), rc(512, 1), rc(1024, 0), rc(1024, 1), slice(R0 + 1536, R0 + 1664), slice(R0 + 1664, R0 + 1792)]
    wR = np.concatenate([w[:, s] for s in cols], axis=1)
    mu = inp["rwkv_mu"][l]
    muR = np.stack([mu[s.start - R0:s.stop - R0] for s in cols], axis=1)
    qi = w[:, 768:1024]
    wO = np.concatenate([w[:, 0:512], qi, w[:, 1056:1064]], axis=1)
    ch = slice(own, own + 256)
    vecs = [inp["rwkv_w0"][l], inp["rwkv_a0"][l], inp["rwkv_k_k"][l], inp["rwkv_k_a"][l], inp["rwkv_r_k"][l].reshape(-1),
            inp["rwkv_gn_g"][l], inp["rwkv_gn_b"][l]]
    pv = np.stack([v[ch].reshape(2, 128) for v in vecs], axis=-1)
    pvec = np.ascontiguousarray(pv.transpose(1, 0, 2).reshape(128, 2 * NPV))
    return dict(wA=np.ascontiguousarray(wA), wR=np.ascontiguousarray(wR), muR=np.ascontiguousarray(muR),
                wO=np.ascontiguousarray(wO), pvec=pvec,
                wup=np.ascontiguousarray(inp["rwkv_w_up"][l][:, ch]), aup=np.ascontiguousarray(inp["rwkv_a_up"][l][:, ch]),
                gup=np.ascontiguousarray(inp["rwkv_g_up"][l][:, ch]), cst=make_cst(half))


def own_rows(hfull, half):
    t = hfull.reshape(NT, 128, -1)
    return np.ascontiguousarray(t[half::2].reshape(TOWN, -1))


def L2_io(k, sfx=""):
    return dict(wg=k.din("wg" + sfx, [D, 2048]), bg=k.din("bg" + sfx, [128, 16]), wba=k.din("wba" + sfx, [512, D]),
                wbb=k.din("wbb" + sfx, [512, D]), wout=k.din("wout" + sfx, [D, D]), lnp=k.din("lnp" + sfx, [4, D]),
                w1=k.din("w1" + sfx, [16, D, 512]), w3=k.din("w3" + sfx, [16, D, 512]), w2=k.din("w2" + sfx, [16, 512, D]))


def build_L2(k=None, io=None, banks=None):
    alone = k is None
    if alone:
        k = KB()
        io = L2_io(k)
        io.update(hown=k.din("hown", [TOWN, D]), oa=k.din("oa", [TOWN, 512]), obT=k.din("obT", [512, TOWN]),
                  wr=k.din("wr", [D, 16]), br=k.din("br", [1, 16]), idn=k.din("idn", [128, 128]),
                  h1s=k.dout("h1s", [TOWN, D]), out=k.dout("h2", [TOWN, D]), obG=None, selw=None)
    S = k.S
    hown, oa_d, obT_d = io["hown"], io["oa"], io["obT"]
    wg_d, bg_d, wba_d, wbb_d, wout_d, lnp_d = io["wg"], io["bg"], io["wba"], io["wbb"], io["wout"], io["lnp"]
    wr_d, br_d, w1_d, w3_d, w2_d, idn_d = io["wr"], io["br"], io["w1"], io["w3"], io["w2"], io["idn"]
    h1s, out_d, obG, selw_d = io["h1s"], io["out"], io["obG"], io["selw"]

    idn = k.sb("idn", [128, 128]); S.dma("sp", idn[:, :], idn_d[:, :], writes=[idn])
    ident = idn.t[:, :]
    gb1 = k.sb("gb1", [128, 2, D]); gb2 = k.sb("gb2", [128, 2, D])
    for j in range(2):
        S.dma("sp", gb1[:, j, :], lnp_d[j:j + 1, :].to_broadcast([128, D]), writes=[gb1])
        S.dma("sp", gb2[:, j, :], lnp_d[2 + j:3 + j, :].to_broadcast([128, D]), writes=[gb2])
    brb = k.sb("brb", [128, 16]); S.dma("sp", brb[:, :], br_d[0:1, :].to_broadcast([128, 16]), writes=[brb])
    wr = k.sb("wr", [128, 8, 16]); S.dma("sp", wr[:, :, :], wr_d.rearrange("(kt p) c -> p kt c", p=128), writes=[wr])
    bg = k.sb("bg", [128, 16]); S.dma("sp", bg[:, :], bg_d[:, :], writes=[bg])
    h1T = k.sb("h1T", [128, 8, TOWN], BF16)
    comb = k.sb("comb", [128, NOWN, 16])
    if banks is None:
        banks = [k.ps(f"bank{i}", [128, 512]) for i in range(8)]
    if obG is not None:
        selw = k.sb("selw", [128, 2]); S.dma("sp", selw[:, :], selw_d[:, :], writes=[selw])
    xt = k.sb("xt", [128, D]); tmp = k.sb("tmp", [128, D]); ot = k.sb("ot", [128, D]); stt_ = k.sb("stt", [128, 4, 6])

    esA = ExitStack()
    wg = k.sb("wg", [128, 8, 2048], BF16, esA); S.dma("pool", wg[:, :, :], wg_d.rearrange("(kt p) c -> p kt c", p=128), writes=[wg])
    wba = k.sb("wba", [128, 4, D], BF16, esA); S.dma("pool", wba[:, :, :], wba_d.rearrange("(kt p) c -> p kt c", p=128), writes=[wba])
    wbb = k.sb("wbb", [128, 4, D], BF16, esA); S.dma("pool", wbb[:, :, :], wbb_d.rearrange("(kt p) c -> p kt c", p=128), writes=[wbb])
    wout = k.sb("wout", [128, 8, D], BF16, esA); S.dma("pool", wout[:, :, :], wout_d.rearrange("(kt p) c -> p kt c", p=128), writes=[wout])
    hst = k.sb("hst", [128, 4, D], F32, esA)
    oast = k.sb("oast", [128, 512], F32, esA)
    hT = k.sb("hT", [128, 8, 512], BF16, esA)
    oaT = k.sb("oaT", [128, 4, 512], BF16, esA)
    obT = k.sb("obT", [128, 4, 512], BF16, esA)
    obc = [k.sb("obc", [128, 4, 512], BF16, esA) for _ in range(2)] if obG is not None else None
    gT = k.sb("gT", [128, 16, 512], BF16, esA)
    mT = k.sb("mT", [128, 8, 512], BF16, esA)
    t1 = k.sb("t1", [128, 512], F32, esA); t2 = k.sb("t2", [128, 512], F32, esA)
    h1f = k.sb("h1f", [128, 8, 128], F32, esA)
    rs = k.sb("rs", [128, 64], F32, esA)
    pad8 = k.sb("pad8", [128, 4, 8], F32, esA); k.memset(pad8[:, :, :], -1e30, [pad8])
    m8 = k.sb("m8", [128, 4, 8], F32, esA)
    for g0 in range(0, NOWN, 4):
        nt = min(4, NOWN - g0)
        N = nt * 128
        gc = slice(g0 * 128, g0 * 128 + N)
        for t in range(nt):
            S.dma("sp", hst[:, t, :], hown[(g0 + t) * 128:(g0 + t + 1) * 128, :], writes=[hst])
            for half in range(2):
                bk = banks[half]
                for q in range(4):
                    kt = half * 4 + q
                    k.tr(bk[:, q * 128:(q + 1) * 128], hst[:, t, kt * 128:(kt + 1) * 128], ident, [hst, idn], [bk], inc=(q == 3))
                k.copy(hT[:, half * 4:half * 4 + 4, t * 128:(t + 1) * 128], bk.t[:, :].rearrange("p (q c) -> p q c", q=4), [bk], [hT], eng="dve")
            S.dma("act", oast[:, :], oa_d[(g0 + t) * 128:(g0 + t + 1) * 128, :], writes=[oast])
            bk = banks[2]
            for q in range(4):
                k.tr(bk[:, q * 128:(q + 1) * 128], oast[:, q * 128:(q + 1) * 128], ident, [oast, idn], [bk], inc=(q == 3))
            k.copy(oaT[:, :, t * 128:(t + 1) * 128], bk.t[:, :].rearrange("p (q c) -> p q c", q=4), [bk], [oaT], eng="dve")
        if obG is None:
            S.dma("pool", obT[:, :, 0:N], obT_d[:, gc].rearrange("(kt p) c -> p kt c", p=128), writes=[obT])
        else:
            for r_ in range(2):
                for R in range(2):
                    for s_ in range(2):
                        S.dma("pool", obc[r_][s_ * 64:(s_ + 1) * 64, 2 * R:2 * R + 2, 0:N], obG[r_, :, s_, R, :, gc].rearrange("ct w c -> w ct c"), writes=[obc[r_]])
            k.ts(obc[0][:, :, 0:N], obc[0][:, :, 0:N], selw[:, 0:1], ALU.mult, [obc[0], selw], [obc[0]])
            k.stt(obT[:, :, 0:N], obc[1][:, :, 0:N], selw[:, 1:2], obc[0][:, :, 0:N], ALU.mult, ALU.add, [obc[1], selw, obc[0]], [obT])
        for m in range(16):
            bk = banks[3 + m % 2]
            for kt in range(8):
                k.mm(bk[:, 0:N], wg[:, kt, m * 128:(m + 1) * 128], hT[:, kt, 0:N], kt == 0, kt == 7, [wg, hT], [bk], inc=(kt == 7))
            k.act(gT[:, m, 0:N], bk[:, 0:N], AF.Sigmoid, [bk, bg], [gT], bias=bg[:, m:m + 1])
        for m in range(8):
            ba, bb = banks[5], banks[6]
            for kt in range(4):
                k.mm(ba[:, 0:N], wba[:, kt, m * 128:(m + 1) * 128], oaT[:, kt, 0:N], kt == 0, kt == 3, [wba, oaT], [ba], inc=(kt == 3))
            for kt in range(4):
                k.mm(bb[:, 0:N], wbb[:, kt, m * 128:(m + 1) * 128], obT[:, kt, 0:N], kt == 0, kt == 3, [wbb, obT], [bb], inc=(kt == 3))
            k.tt(t1[:, 0:N], ba[:, 0:N], gT[:, m, 0:N], ALU.mult, [ba, gT], [t1])
            k.tt(t2[:, 0:N], bb[:, 0:N], gT[:, 8 + m, 0:N], ALU.mult, [bb, gT], [t2])
            k.tt(mT[:, m, 0:N], t1[:, 0:N], t2[:, 0:N], ALU.add, [t1, t2], [mT], eng="pool")
        for t in range(nt):
            i = g0 + t
            for half in range(2):
                bk = banks[half]
                for kt in range(8):
                    k.mm(bk[:, :], mT[:, kt, t * 128:(t + 1) * 128], wout[:, kt, half * 512:(half + 1) * 512], kt == 0, kt == 7, [mT, wout], [bk], inc=(kt == 7))
                k.stt(xt[:, half * 512:(half + 1) * 512], hst[:, t, half * 512:(half + 1) * 512], ALPHA, bk[:, :], ALU.mult, ALU.add, [hst, bk], [xt])
            layer_norm_tile(k, xt, gb1, ot, tmp, stt_)
            S.dma("sp", h1s[i * 128:(i + 1) * 128, :], ot[:, :], reads=[ot])
            for half in range(2):
                bk = banks[2 + half]
                for q in range(4):
                    kt = half * 4 + q
                    k.tr(bk[:, q * 128:(q + 1) * 128], ot[:, kt * 128:(kt + 1) * 128], ident, [ot, idn], [bk], inc=(q == 3))
                k.copy(h1T[:, half * 4:half * 4 + 4, i * 128:(i + 1) * 128], bk.t[:, :].rearrange("p (q c) -> p q c", q=4), [bk], [h1T], eng="dve")
                k.copy(h1f[:, half * 4:half * 4 + 4, :], bk.t[:, :].rearrange("p (q c) -> p q c", q=4), [bk], [h1f], eng="dve")
            bk = banks[7]
            for kt in range(8):
                k.mm(bk[:, 0:16], h1f[:, kt, :], wr[:, kt, :], kt == 0, kt == 7, [h1f, wr], [bk], inc=(kt == 7))
            sg = rs.t[:, 0:16]; sel = rs.t[:, 16:32]
            k.act(sg, bk[:, 0:16], AF.Sigmoid, [bk], [rs])
            k.tt(sel, sg, brb[:, :], ALU.add, [rs, brb], [rs])
            k.copy(pad8[:, :, 0:4], sel.rearrange("p (g e) -> p g e", g=4), [rs], [pad8], eng="dve")
            for g in range(4):
                S.op("dve", lambda e, o=m8[:, g, :], i_=pad8[:, g, :]: e.max(out=o, in_=i_), reads=[pad8], writes=[m8])
            gsc = rs.t[:, 32:36]; gmx = rs.t[:, 36:37]; goh = rs.t[:, 40:44]; msk = rs.t[:, 44:60]; den = rs.t[:, 60:61]
            k.tt(gsc, m8[:, :, 0], m8[:, :, 1], ALU.add, [m8], [rs])
            S.op("dve", lambda e, o=gmx, i_=gsc: e.tensor_reduce(out=o, in_=i_, axis=AX.X, op=ALU.max), reads=[rs], writes=[rs])
            k.ts(goh, gsc, gmx, ALU.is_ge, [rs], [rs])
            msk3 = msk.rearrange("p (g e) -> p g e", g=4)
            k.tt(msk3, sel.rearrange("p (g e) -> p g e", g=4), m8[:, :, 1:2].to_broadcast([128, 4, 4]), ALU.is_ge, [rs, m8], [rs])
            k.tt(msk3, msk3, goh.unsqueeze(2).to_broadcast([128, 4, 4]), ALU.mult, [rs], [rs])
            k.tt(msk, msk, sg, ALU.mult, [rs], [rs])
            S.op("dve", lambda e, o=den, i_=msk: e.tensor_reduce(out=o, in_=i_, axis=AX.X, op=ALU.add), reads=[rs], writes=[rs])
            k.recip(den, den, [rs], [rs])
            k.ts(comb[:, i, :], msk, den, ALU.mult, [rs], [comb])
    S.barrier()
    esA.close()

    esB = ExitStack()
    yacc = k.sb("yacc", [128, NOWN, D], F32, esB)
    k.memset(yacc[:, :, :], 0.0, [yacc])
    w1s = [k.sb("w1s", [128, 8, 512], BF16, esB) for _ in range(2)]
    w3s = [k.sb("w3s", [128, 8, 512], BF16, esB) for _ in range(2)]
    w2s = [k.sb("w2s", [128, 4, D], BF16, esB) for _ in range(2)]
    z = k.sb("z", [128, 4, 512], BF16, esB)
    sl = k.sb("sl", [128, 512], F32, esB)
    for e_ in range(16):
        p = e_ % 2
        S.dma("pool", w1s[p][:, :, :], w1_d[e_].rearrange("(kt p) c -> p kt c", p=128), writes=[w1s[p]])
        S.dma("pool", w3s[p][:, :, :], w3_d[e_].rearrange("(kt p) c -> p kt c", p=128), writes=[w3s[p]])
        S.dma("pool", w2s[p][:, :, :], w2_d[e_].rearrange("(kt p) c -> p kt c", p=128), writes=[w2s[p]])
        for g0 in range(0, NOWN, 4):
            nt = min(4, NOWN - g0)
            N = nt * 128
            gc = slice(g0 * 128, g0 * 128 + N)
            for m in range(4):
                ba, bc = banks[(2 * m) % 4], banks[(2 * m) % 4 + 1]
                for kt in range(8):
                    k.mm(ba[:, 0:N], w1s[p][:, kt, m * 128:(m + 1) * 128], h1T[:, kt, gc], kt == 0, kt == 7, [w1s[p], h1T], [ba], inc=(kt == 7))
                for kt in range(8):
                    k.mm(bc[:, 0:N], w3s[p][:, kt, m * 128:(m + 1) * 128], h1T[:, kt, gc], kt == 0, kt == 7, [w3s[p], h1T], [bc], inc=(kt == 7))
                k.act(sl[:, 0:N], ba[:, 0:N], AF.Silu, [ba], [sl])
                k.tt(z[:, m, 0:N], sl[:, 0:N], bc[:, 0:N], ALU.mult, [sl, bc], [z])
            for t in range(nt):
                i = g0 + t
                for half in range(2):
                    bk = banks[4 + (2 * t + half) % 4]
                    for m in range(4):
                        k.mm(bk[:, :], z[:, m, t * 128:(t + 1) * 128], w2s[p][:, m, half * 512:(half + 1) * 512], m == 0, m == 3, [z, w2s[p]], [bk], inc=(m == 3))
                    ya = yacc[:, i, half * 512:(half + 1) * 512]
                    k.stt(ya, bk[:, :], comb[:, i, e_:e_ + 1], ya, ALU.mult, ALU.add, [bk, comb, yacc], [yacc])
            if e_ == 15:
                for t in range(nt):
                    i = g0 + t
                    S.dma("sp", xt[:, :], h1s[i * 128:(i + 1) * 128, :], writes=[xt])
                    k.stt(xt[:, :], xt[:, :], ALPHA, yacc[:, i, :], ALU.mult, ALU.add, [xt, yacc], [xt])
                    layer_norm_tile(k, xt, gb2, ot, tmp, stt_)
                    S.dma("act", out_d[i * 128:(i + 1) * 128, :], ot[:, :], reads=[ot])
    S.barrier()
    esB.close()
    if alone:
        return k.done()


def prep_L2(l, inp):
    w = inp["w_in"][l]
    bgv = inp["b_gate"][l].reshape(16, 128).T
    lnp = np.stack([inp["ln1_g"][l], inp["ln1_b"][l], inp["ln2_g"][l], inp["ln2_b"][l]], axis=0)
    return dict(wg=np.ascontiguousarray(w[:, 2856:4904]), bg=np.ascontiguousarray(bgv),
                wba=np.ascontiguousarray(inp["w_branch_a"][l]), wbb=np.ascontiguousarray(inp["w_branch_b"][l]),
                wout=np.ascontiguousarray(inp["w_out"][l]), lnp=np.ascontiguousarray(lnp),
                wr=np.ascontiguousarray(inp["w_router"]), br=np.ascontiguousarray(inp["b_router"].reshape(1, 16)),
                w1=np.ascontiguousarray(inp["w_exp1"][l]), w3=np.ascontiguousarray(inp["w_exp3"][l]),
                w2=np.ascontiguousarray(inp["w_exp2"][l]), idn=np.eye(128, dtype=np.float32))


PAIRS = [[0, 1], [2, 3], [4, 5], [6, 7]]


def build_fused():
    k = KB()
    S = k.S
    nc = k.nc
    xown = k.din("xown", [TOWN, D])
    lng = k.din("lng", [1, D]); lnb = k.din("lnb", [1, D])
    cst_d = k.din("cst", [128, C_END])
    selw_d = k.din("selw", [128, 2])
    wr_d = k.din("wr", [D, 16]); br_d = k.din("br", [1, 16]); idn_d = k.din("idn", [128, 128])
    io1 = [L1_io(k, str(l)) for l in range(2)]
    io2 = [L2_io(k, str(l)) for l in range(2)]
    out_d = k.dout("out", [TOWN, D])
    hO = nc.dram_tensor("hO_i", [TOWN, D], F32).ap()
    hAg = nc.dram_tensor("hAg_i", [2 * TOWN, D], F32).ap()
    oaS = nc.dram_tensor("oaS_i", [TOWN, 512], F32).ap()
    obX = nc.dram_tensor("obX_i", [2 * 256, TOWN], F32).ap()
    obGt = nc.dram_tensor("obG_i", [2 * 2 * 256, TOWN], F32).ap()
    h1s = nc.dram_tensor("h1s_i", [TOWN, D], F32).ap()
    obX3 = obX.rearrange("(r c) t -> r c t", r=2)
    obG6 = obGt.rearrange("(rp ct s R w) t -> rp ct s R w t", rp=2, ct=2, s=2, R=2, w=64)
    banks = [k.ps(f"bank{i}", [128, 512]) for i in range(8)]

    def phase(fn):
        es = ExitStack()
        k.esd = es
        fn()
        S.barrier()
        es.close()
        k.esd = k.es

    def gather(src, dst, nrows, cr):
        r0 = 0
        while r0 < nrows:
            n = min(cr, nrows - r0)
            S.coll(lambda e, r0=r0, n=n: e.collective_compute("AllGather", op=ALU.bypass, replica_groups=PAIRS,
                                                             ins=[src[r0:r0 + n, :]], outs=[dst[2 * r0:2 * r0 + 2 * n, :]]))
            r0 += n
        S.barrier()

    def hag_row(j):
        r, i = j % 2, j // 2
        q = i // 2
        return (q * 512 + r * 256 + (i % 2) * 128) if q < 8 else (4096 + r * 128)

    phase(lambda: build_L0(k, dict(x=xown, g=lng, b=lnb, y=hO)))
    for l in range(2):
        gather(hO, hAg, TOWN, 256)
        d1 = dict(io1[l])
        d1.update(hown=hO, cst=cst_d, oa=oaS,
                  hin_tile=lambda j: hAg[hag_row(j):hag_row(j) + 128, :],
                  ob_out=lambda ct, j: obX3[j % 2, ct * 128:(ct + 1) * 128, (j // 2) * 128:(j // 2) * 128 + 128])
        phase(lambda: build_L1_full(k, d1, banks))
        gather(obX, obGt, 512, 64)
        d2 = dict(io2[l])
        d2.update(hown=hO, oa=oaS, obT=None, wr=wr_d, br=br_d, idn=idn_d, h1s=h1s,
                  out=(out_d if l == 1 else hO), obG=obG6, selw=selw_d)
        phase(lambda: build_L2(k, d2, banks))
    return k.done()


_CACHE = {}


def kernel(**inp):
    inp = {k_: np.asarray(v, dtype=np.float32) for k_, v in inp.items()}
    x = inp["x"]
    B = x.shape[0]
    cores = list(range(8))
    full = np.zeros((B, LP, D), np.float32)
    full[:, :16] = inp["meta_tokens"][None]
    full[:, 16:LR] = x
    if "F" not in _CACHE:
        _CACHE["F"] = build_fused()
    p1 = [[prep_L1(l, inp, half) for half in range(2)] for l in range(2)]
    p2 = [prep_L2(l, inp) for l in range(2)]
    maps = []
    for c in cores:
        b, half = c // 2, c % 2
        m = dict(xown=own_rows(full[b], half), lng=np.ascontiguousarray(inp["ln_in_g"].reshape(1, D)),
                 lnb=np.ascontiguousarray(inp["ln_in_b"].reshape(1, D)), cst=p1[0][half]["cst"],
                 selw=np.ascontiguousarray(np.tile(np.eye(2, dtype=np.float32)[half][None, :], (128, 1))),
                 wr=p2[0]["wr"], br=p2[0]["br"], idn=p2[0]["idn"])
        for l in range(2):
            for nm in ("wA", "wR", "muR", "wO", "pvec", "wup", "aup", "gup"):
                m[nm + str(l)] = p1[l][half][nm]
            for nm in ("wg", "bg", "wba", "wbb", "wout", "lnp", "w1", "w3", "w2"):
                m[nm + str(l)] = p2[l][nm]
        maps.append(m)
    res = run_bass_kernel_spmd(_CACHE["F"], maps, core_ids=cores).results
    h = np.zeros((B, LP, D), np.float32)
    for c in cores:
        b, half = c // 2, c % 2
        h[b].reshape(NT, 128, D)[half::2] = res[c]["out"].reshape(NOWN, 128, D)
    return np.ascontiguousarray(h[:, 16:LR])
```

```python
import numpy as np
from contextlib import ExitStack
import concourse.bass as bass
import concourse.mybir as mybir
from concourse.bass_utils import run_bass_kernel_spmd

F32 = mybir.dt.float32
BF16 = mybir.dt.bfloat16
AF = mybir.ActivationFunctionType
ALU = mybir.AluOpType
AX = mybir.AxisListType

D = 1024
LR = 4112
NT = 34
LP = NT * 128
NOWN = 17
TOWN = NOWN * 128
SEGS = [(s * 512, 512) for s in range(8)] + [(4096, 256)]
ALPHA = 4.0 ** 0.25
LN_EPS = 1e-5
GN_EPS = 64e-5
NEG = -30000.0
NBIS = 20
UO = 4
DBG = {}
CDEC = float(np.exp(-0.5))


class Buf:
    __slots__ = ("w", "r", "name")

    def __init__(self, name=""):
        self.w = None
        self.r = {}
        self.name = name


class Tl:
    def __init__(self, t, name):
        self.t = t
        self.b = Buf(name)

    def __getitem__(self, idx):
        return self.t[idx]


class Sched:
    ENGS = ["pe", "act", "dve", "pool", "sp"]
    QS = ["sp", "act", "pool"]
    NSLOT = 8

    def __init__(self, nc, es):
        self.nc = nc
        self.prog = {e: [] for e in self.ENGS}
        self.cnt = {e: 0 for e in self.ENGS}
        self.known = {e: {} for e in self.ENGS}
        self.sems = {}
        for e in self.ENGS:
            self.sems[("c", e)] = es.enter_context(nc.semaphore("c_" + e))
        self.dcnt = {}
        for q in self.QS:
            self.dcnt[q] = 0
            for s in range(self.NSLOT):
                self.sems[("d", q, s)] = es.enter_context(nc.semaphore(f"d_{q}_{s}"))
        self.nins = 0
        self.sems[("cc",)] = es.enter_context(nc.semaphore("cc_sem"))
        self.ccnt = 0

    def coll(self, fn, reads=(), writes=()):
        reads = [x.b if isinstance(x, Tl) else x for x in reads]
        writes = [x.b if isinstance(x, Tl) else x for x in writes]
        self._wait("pool", self._deps("pool", reads, writes))
        self.ccnt += 1
        sem = self.sems[("cc",)]
        self.prog["pool"].append(lambda eng, fn=fn, sem=sem: fn(eng).then_inc(sem, 1))
        self._mark((("cc",), self.ccnt), reads, writes)

    def _wait(self, e, deps):
        kn = self.known[e]
        for key, val in deps.items():
            if kn.get(key, 0) >= val:
                continue
            kn[key] = val
            sem = self.sems[key]
            self.prog[e].append(lambda eng, sem=sem, val=val: eng.wait_ge(sem, val))

    def _deps(self, e, reads, writes):
        deps = {}

        def add(k, v):
            if e == "pe" and k == ("c", "pe"):
                return
            if deps.get(k, 0) < v:
                deps[k] = v

        for b in reads:
            if b.w is not None:
                add(*b.w)
        for b in writes:
            if b.w is not None:
                add(*b.w)
            for k, v in b.r.items():
                add(k, v)
        return deps

    def _mark(self, tok, reads, writes):
        k, v = tok
        for b in reads:
            if b.r.get(k, 0) < v:
                b.r[k] = v
        for b in writes:
            b.w = tok
            b.r = {}

    def op(self, e, fn, reads=(), writes=(), inc=True):
        reads = [x.b if isinstance(x, Tl) else x for x in reads]
        writes = [x.b if isinstance(x, Tl) else x for x in writes]
        self._wait(e, self._deps(e, reads, writes))
        self.nins += 1
        if inc:
            self.cnt[e] += 1
            tok = (("c", e), self.cnt[e])
            sem = self.sems[("c", e)]
            self.prog[e].append(lambda eng, fn=fn, sem=sem: fn(eng).then_inc(sem, 1))
        else:
            tok = (("c", e), self.cnt[e] + 1)
            self.prog[e].append(lambda eng, fn=fn: fn(eng))
        self._mark(tok, reads, writes)

    def dma(self, q, out, in_, reads=(), writes=(), fn=None):
        reads = [x.b if isinstance(x, Tl) else x for x in reads]
        writes = [x.b if isinstance(x, Tl) else x for x in writes]
        i = self.dcnt[q]
        self.dcnt[q] += 1
        slot = i % self.NSLOT
        key = ("d", q, slot)
        deps = self._deps(q, reads, writes)
        if i >= self.NSLOT:
            prev = 16 * (i // self.NSLOT)
            if deps.get(key, 0) < prev:
                deps[key] = prev
        self._wait(q, deps)
        val = 16 * (i // self.NSLOT + 1)
        sem = self.sems[key]
        self.nins += 1
        if fn is not None:
            self.prog[q].append(lambda eng, fn=fn, sem=sem: fn(eng).then_inc(sem, 16))
        else:
            self.prog[q].append(lambda eng, out=out, in_=in_, sem=sem: eng.dma_start(out=out, in_=in_).then_inc(sem, 16))
        self._mark((key, val), reads, writes)

    def _alldeps(self):
        deps = {}
        for q in self.QS:
            n = self.dcnt[q]
            for s in range(self.NSLOT):
                c = (n - s + self.NSLOT - 1) // self.NSLOT if n > s else 0
                if c > 0:
                    deps[("d", q, s)] = 16 * c
        for e in self.ENGS:
            if self.cnt[e] > 0:
                deps[("c", e)] = self.cnt[e]
        if self.ccnt > 0:
            deps[("cc",)] = self.ccnt
        return deps

    def barrier(self):
        deps = self._alldeps()
        for e in self.ENGS:
            d = {k: v for k, v in deps.items() if k != ("c", e)}
            self._wait(e, d)

    def finish(self):
        self.barrier()

    def emit(self):
        nc = self.nc
        with nc.Block() as block:
            @block.tensor
            def _(eng):
                for f in self.prog["pe"]:
                    f(eng)

            @block.scalar
            def _(eng):
                for f in self.prog["act"]:
                    f(eng)

            @block.vector
            def _(eng):
                for f in self.prog["dve"]:
                    f(eng)

            @block.gpsimd
            def _(eng):
                for f in self.prog["pool"]:
                    f(eng)

            @block.sync
            def _(eng):
                for f in self.prog["sp"]:
                    f(eng)


class KB:
    def __init__(self):
        self.nc = bass.Bass("TRN2", target_bir_lowering=False)
        self.es = ExitStack()
        self.S = Sched(self.nc, self.es)
        self.n = 0
        self.rr = 0
        self.esd = self.es

    def din(self, name, shape, dt=F32):
        return self.nc.dram_tensor(name, list(shape), dt, kind="ExternalInput").ap()

    def dout(self, name, shape, dt=F32):
        return self.nc.dram_tensor(name, list(shape), dt, kind="ExternalOutput").ap()

    def sb(self, name, shape, dt=F32, es=None):
        self.n += 1
        nm = f"{name}_{self.n}"
        return Tl((es or self.esd).enter_context(self.nc.sbuf_tensor(nm, list(shape), dt)), nm)

    def ps(self, name, shape, dt=F32, es=None):
        self.n += 1
        nm = f"{name}_{self.n}"
        return Tl((es or self.esd).enter_context(self.nc.psum_tensor(nm, list(shape), dt)), nm)

    def mm(self, out, lhsT, rhs, start, stop, reads, writes, inc=True, skip=False):
        self.S.op("pe", lambda e: e.matmul(out, lhsT=lhsT, rhs=rhs, start=start, stop=stop, skip_group_check=skip), reads=reads, writes=writes, inc=inc)

    def tr(self, out, in_, ident, reads, writes, inc=True):
        self.S.op("pe", lambda e: e.transpose(out=out, in_=in_, identity=ident), reads=reads, writes=writes, inc=inc)

    def act(self, out, in_, func, reads, writes, bias=None, scale=1.0, accum=None):
        kw = {}
        if bias is not None:
            kw["bias"] = bias
        if accum is not None:
            kw["accum_out"] = accum
        self.S.op("act", lambda e: e.activation(out=out, in_=in_, func=func, scale=scale, **kw), reads=reads, writes=writes)

    def tt(self, out, in0, in1, op, reads, writes, eng="dve"):
        self.S.op(eng, lambda e: e.tensor_tensor(out=out, in0=in0, in1=in1, op=op), reads=reads, writes=writes)

    def ts(self, out, in0, s1, op0, reads, writes, s2=None, op1=None, accum=None, eng="dve"):
        kw = {}
        if op1 is not None:
            kw["op1"] = op1
        if accum is not None:
            kw["accum_out"] = accum
        self.S.op(eng, lambda e: e.tensor_scalar(out=out, in0=in0, scalar1=s1, scalar2=s2, op0=op0, **kw), reads=reads, writes=writes)

    def stt(self, out, in0, scalar, in1, op0, op1, reads, writes):
        self.S.op("dve", lambda e: e.scalar_tensor_tensor(out=out, in0=in0, scalar=scalar, in1=in1, op0=op0, op1=op1), reads=reads, writes=writes)

    def copy(self, out, in_, reads, writes, eng=None):
        if eng is None:
            self.rr += 1
            eng = "act" if self.rr % 2 else "dve"
        if eng == "act":
            self.S.op("act", lambda e: e.activation(out=out, in_=in_, func=AF.Copy), reads=reads, writes=writes)
        else:
            self.S.op(eng, lambda e: e.tensor_copy(out=out, in_=in_), reads=reads, writes=writes)

    def recip(self, out, in_, reads, writes):
        self.S.op("dve", lambda e: e.reciprocal(out=out, in_=in_), reads=reads, writes=writes)

    def memset(self, ap, val, writes, eng="pool"):
        self.S.op(eng, lambda e: e.memset(ap, val), writes=writes)

    def done(self):
        self.S.finish()
        self.S.emit()
        self.es.close()
        return self.nc


def layer_norm_tile(k, x, gb, out, tmp, stat, reads_extra=()):
    nchunk = 2
    st = stat
    for c in range(nchunk):
        k.S.op("dve", lambda e, c=c: e.bn_stats(out=st[:, c, :], in_=x[:, c * 512:(c + 1) * 512]), reads=[x], writes=[st])
    k.S.op("dve", lambda e: e.bn_aggr(out=st[:, 2, 0:2], in_=st[:, 0:2, :]), reads=[st], writes=[st])
    k.ts(st[:, 3, 0:1], st[:, 2, 1:2], LN_EPS, ALU.add, [st], [st])
    k.act(st[:, 3, 1:2], st[:, 3, 0:1], AF.Sqrt, [st], [st])
    k.S.op("dve", lambda e: e.reciprocal(out=st[:, 3, 2:3], in_=st[:, 3, 1:2]), reads=[st], writes=[st])
    k.ts(tmp[:, :], x[:, :], st[:, 2, 0:1], ALU.subtract, [x, st], [tmp], s2=st[:, 3, 2:3], op1=ALU.mult)
    k.tt(tmp[:, :], tmp[:, :], gb[:, 0, :], ALU.mult, [tmp, gb], [tmp], eng="pool")
    k.tt(out[:, :], tmp[:, :], gb[:, 1, :], ALU.add, [tmp, gb], [out])


def build_L0(k=None, io=None):
    alone = k is None
    if alone:
        k = KB()
        io = dict(x=k.din("x", [TOWN, D]), g=k.din("g", [1, D]), b=k.din("b", [1, D]), y=k.dout("y", [TOWN, D]))
    x, g, b, y = io["x"], io["g"], io["b"], io["y"]
    gb = k.sb("gb", [128, 2, D])
    k.S.dma("sp", gb[:, 0, :], g[0:1, :].to_broadcast([128, D]), writes=[gb])
    k.S.dma("sp", gb[:, 1, :], b[0:1, :].to_broadcast([128, D]), writes=[gb])
    xs = [k.sb("x", [128, D]) for _ in range(2)]
    ts_ = [k.sb("t", [128, D]) for _ in range(2)]
    os_ = [k.sb("o", [128, D]) for _ in range(2)]
    sts = [k.sb("st", [128, 4, 6]) for _ in range(2)]
    for i in range(NOWN):
        p = i % 2
        k.S.dma("sp", xs[p][:, :], x[i * 128:(i + 1) * 128, :], writes=[xs[p]])
        layer_norm_tile(k, xs[p], gb, os_[p], ts_[p], sts[p])
        k.S.dma("act", y[i * 128:(i + 1) * 128, :], os_[p][:, :], reads=[os_[p]])
    if alone:
        return k.done()
    k.S.barrier()


C_ID = 0
C_BD = 128
C_MAB = 256
C_MC = 384
C_RST = 448
C_ADM = 960
C_MAB4 = 1344
C_MC4 = 1856
C_ID4 = 2112
C_END = 2368
NPV = 7


def L1_io(k, sfx=""):
    return dict(wA=k.din("wA" + sfx, [D, 480]), wR=k.din("wR" + sfx, [D, 1024]), muR=k.din("muR" + sfx, [128, 8]),
                wO=k.din("wO" + sfx, [D, 776]), pvec=k.din("pvec" + sfx, [128, 2 * NPV]), wup=k.din("wup" + sfx, [64, 256]),
                aup=k.din("aup" + sfx, [64, 256]), gup=k.din("gup" + sfx, [128, 256]))


def build_L1(k=None, io=None, banks=None):
    alone = k is None
    if alone:
        k = KB()
        io = L1_io(k)
        hin_ = k.din("hin", [LP, D])
        obT_ = k.dout("obT", [256, LP])
        io.update(hown=k.din("hown", [TOWN, D]), cst=k.din("cst", [128, C_END]), oa=k.dout("oa", [TOWN, 512]),
                  hin_tile=lambda j: hin_[j * 128:(j + 1) * 128, :],
                  ob_out=lambda ct, j: obT_[ct * 128:(ct + 1) * 128, j * 128:(j + 1) * 128])
    S = k.S
    hown = io["hown"]
    wA_d, wR_d, muR_d, wO_d, pvec_d = io["wA"], io["wR"], io["muR"], io["wO"], io["pvec"]
    wup_d, aup_d, gup_d, cst_d, oa_d = io["wup"], io["aup"], io["gup"], io["cst"], io["oa"]
    hin_tile, ob_out = io["hin_tile"], io["ob_out"]
    hin = None

    cst = k.sb("cst", [128, C_END])
    S.dma("sp", cst[:, :], cst_d[:, :], writes=[cst])
    identb = k.sb("identb", [128, 128], BF16)
    k.copy(identb[:, :], cst[:, C_ID:C_ID + 128], [cst], [identb], eng="dve")
    ident = cst.t[:, C_ID:C_ID + 128]
    bdones = cst.t[:, C_BD:C_BD + 128]
    pvec = k.sb("pvec", [128, 2, NPV + 1])
    S.dma("sp", pvec[:, :, 0:NPV], pvec_d.rearrange("p (c v) -> p c v", c=2), writes=[pvec])
    k.ts(pvec[:, :, NPV:NPV + 1], pvec[:, :, 3:4], -1.0, ALU.mult, [pvec], [pvec], s2=1.0, op1=ALU.add)
    muR = k.sb("muR", [128, 8])
    S.dma("sp", muR[:, :], muR_d[:, :], writes=[muR])
    wup = k.sb("wup", [64, 256]); S.dma("act", wup[:, :], wup_d[:, :], writes=[wup])
    aup = k.sb("aup", [128, 256]); S.dma("act", aup[64:128, :], aup_d[:, :], writes=[aup])
    gup = k.sb("gup", [128, 256]); S.dma("act", gup[:, :], gup_d[:, :], writes=[gup])

    kT = k.sb("kT", [128, 2, LP], BF16)
    kiT = k.sb("kiT", [96, LP], BF16)
    Vaug = k.sb("Vaug", [128, NT, 2, 65], BF16)
    k.memset(Vaug[:, :, :, 64:65], 1.0, [Vaug])

    if banks is None:
        banks = [k.ps(f"bank{i}", [128, 512]) for i in range(8)]

    es1 = ExitStack()
    wA = k.sb("wA", [128, 8, 480], BF16, es1)
    wR = k.sb("wR", [128, 8, 1024], BF16, es1)
    S.dma("pool", wA[:, :, :], wA_d.rearrange("(kt p) c -> p kt c", p=128), writes=[wA])
    for kt in range(8):
        S.dma("pool", wR[:, kt, :], wR_d[kt * 128:(kt + 1) * 128, :], writes=[wR])
    hst = [k.sb("hst", [128, D], F32, es1)] * 2
    hT = k.sb("hT", [128, 8, 512], BF16, es1)
    usb = k.sb("usb", [128, 8, 516], F32, es1)
    k.memset(usb[:, :, UO - 1:UO], 0.0, [usb])
    def r1(name, shape=(128, 512)):
        return k.sb(name, list(shape), F32, es1)
    tw = r1("tw", (64, 512)); sgx = r1("sgx")
    ld = r1("ld"); Lc = r1("Lc"); av = r1("av"); kk = r1("kk"); sq = r1("sq"); rn = r1("rn"); kp = r1("kp"); bv = r1("bv")
    E = r1("E"); Einv = r1("Einv"); Eprev = av; Eend = ld; tmpa = sq; dtmp = rn
    gam = k.sb("gam", [128, 2, 8], F32, es1)
    RD = BF16 if DBG.get("rbf16", 0) else F32
    AR = k.sb("AR", [128, 2, 8, 128], RD, es1)
    bt = k.sb("bt", [128, 2, 512], RD, es1)
    kt_ = k.sb("kt", [128, 2, 512], RD, es1)
    Kh = k.sb("Kh", [128, 2, 512], RD, es1)
    Bh = k.sb("Bh", [128, 2, 512], RD, es1)
    KhTM = k.sb("KhTM", [64, 8, 256], RD, es1)
    BhTM = k.sb("BhTM", [64, 8, 256], RD, es1)
    Vtm = k.sb("Vtm", [64, 8, 256], RD, es1)
    Vpad = k.sb("Vpad", [64, 4, 128], RD, es1)
    k.memset(Vpad[:, :, :], 0.0, [Vpad])
    Upad = k.sb("Upad", [64, 4, 128], RD, es1)
    k.memset(Upad[:, :, :], 0.0, [Upad])
    Usb = k.sb("Usb", [64, 256], RD, es1)
    Wsb = k.sb("Wsb", [64, 256], RD, es1)
    SA = [k.sb("SA", [64, 4, 128], RD, es1) for _ in range(2)]
    SB = [k.sb("SB", [64, 4, 128], RD, es1) for _ in range(2)]
    X = [k.sb("X", [64, 4, 64], F32, es1) for _ in range(2)]
    XB = [k.sb("XB", [64, 4, 64], RD, es1) for _ in range(2)] if RD != F32 else X
    PQ = [[k.sb("PQ", [64, 2, 4, 64], RD, es1) for _ in range(2)]] * 2
    Hbd = k.sb("Hbd", [128, 2, 128], F32, es1)
    k.memset(Hbd[:, :, :], 0.0, [Hbd])
    if RD != F32:
        Hb = k.sb("Hb", [128, 2, 128], RD, es1)
        k.memset(Hb[:, :, :], 0.0, [Hb])
    else:
        Hb = Hbd
    identR = identb.t[:, :] if DBG.get("rbf16", 0) else ident
    yT = k.sb("yT", [128, 2, 512], F32, es1)
    bon = k.sb("bon", [128, 2, 512], F32, es1)
    gv = k.sb("gv", [128, 2, 512], F32, es1)
    ob = yT
    id64 = cst.t[0:64, C_ID:C_ID + 64]

    tile_ctr = [0]

    def load_hT(src, row0, ntiles, dst, reads_dst=()):
        for t in range(ntiles):
            i = tile_ctr[0]; tile_ctr[0] += 1
            hs = hst[i % 2]
            S.dma("sp" if i % 2 == 0 else "act", hs[:, :], hin_tile(row0 // 128 + t), writes=[hs])
            for half in range(2):
                bk = banks[half]
                for q in range(4):
                    kt = half * 4 + q
                    k.tr(bk[:, q * 128:(q + 1) * 128], hs[:, kt * 128:(kt + 1) * 128], ident, [hs, cst], [bk], inc=(q == 3))
                k.copy(dst[:, half * 4:half * 4 + 4, t * 128:(t + 1) * 128], bk.t[:, :].rearrange("p (q c) -> p q c", q=4), [bk], [dst])

    def proj_cm(dst_ap, dst_tl, w, c0, M, N, bank, extra_reads=()):
        for kt in range(8):
            k.mm(bank[0:M, 0:N], w[:, kt, c0:c0 + M], hT[:, kt, 0:N], kt == 0, kt == 7, [w, hT], [bank], inc=(kt == 7))
        k.copy(dst_ap, bank[0:M, 0:N], [bank], [dst_tl])

    for (t0, N) in SEGS:
        ntl = N // 128
        nch = N // 64
        load_hT(hin, t0, ntl, hT)
        for g in range(2):
            proj_cm(kT[:, g, t0:t0 + N], kT, wA, g * 128, 128, N, banks[2 + g])
        proj_cm(kiT[:, t0:t0 + N], kiT, wA, 256, 96, N, banks[4])
        for t in range(ntl):
            bk = banks[5 + t % 2]
            for kt in range(8):
                k.mm(bk[:, 0:128], hT[:, kt, t * 128:(t + 1) * 128], wA[:, kt, 352:480], kt == 0, kt == 7, [hT, wA], [bk], inc=(kt == 7))
            k.copy(Vaug[:, t0 // 128 + t, :, 0:64], bk.t[:, 0:128].rearrange("p (g d) -> p g d", g=2), [bk], [Vaug])
        if not DBG.get("rwkv", 1):
            continue
        for m in range(8):
            proj_cm(usb[:, m, UO:UO + N], usb, wR, m * 128, 128, N, banks[2 + m % 4])
        for m in range(8):
            k.tt(dtmp[:, 0:N], usb[:, m, UO - 1:UO - 1 + N], usb[:, m, UO:UO + N], ALU.subtract, [usb], [dtmp])
            k.copy(usb[:, m, UO - 1:UO], usb[:, m, UO + N - 1:UO + N], [usb], [usb], eng="pool")
            k.stt(usb[:, m, UO:UO + N], dtmp[:, 0:N], muR[:, m:m + 1], usb[:, m, UO:UO + N], ALU.mult, ALU.add, [dtmp, muR, usb], [usb])
        if DBG.get("rstage", 9) < 1:
            continue
        def U(m, lo=0, hi=128):
            return usb[lo:hi, m, UO:UO + N]
        k.act(tw[:, 0:N], U(6, 0, 64), AF.Tanh, [usb], [tw])
        k.act(sgx[:, 0:N], U(7), AF.Sigmoid, [usb], [sgx])
        if DBG.get("rstage", 9) < 2:
            continue
        for ct in range(2):
            pv = lambda j: pvec[:, ct, j:j + 1]
            cs = slice(ct * 128, (ct + 1) * 128)
            b0, b1, b2 = banks[2], banks[3], banks[4]
            k.mm(b0[:, 0:N], wup[:, cs], tw[:, 0:N], True, True, [wup, tw], [b0])
            k.act(ld[:, 0:N], b0[:, 0:N], AF.Sigmoid, [b0, pvec], [ld], bias=pv(0))
            S.op("dve", lambda e, o=Lc[:, 0:N], d0=cst[:, C_RST:C_RST + N], d1=ld[:, 0:N]: e.tensor_tensor_scan(out=o, data0=d0, data1=d1, initial=0.0, op0=ALU.mult, op1=ALU.add), reads=[cst, ld], writes=[Lc])
            k.mm(b1[:, 0:N], aup[64:128, cs], U(6, 64, 128), True, True, [aup, usb], [b1])
            k.act(av[:, 0:N], b1[:, 0:N], AF.Sigmoid, [b1, pvec], [av], bias=pv(1))
            k.mm(b2[:, 0:N], gup[:, cs], sgx[:, 0:N], True, True, [gup, sgx], [b2])
            k.copy(gv[:, ct, 0:N], b2[:, 0:N], [b2], [gv], eng="dve")
            k.ts(kk[:, 0:N], U(2 + ct), pv(2), ALU.mult, [usb, pvec], [kk])
            k.act(sq[:, 0:N], kk[:, 0:N], AF.Square, [kk], [sq])
            k.mm(b0[:, 0:N], bdones, sq[:, 0:N], True, True, [cst, sq], [b0])
            k.ts(rn[:, 0:N], b0[:, 0:N], 1e-24, ALU.max, [b0], [rn])
            k.act(rn[:, 0:N], rn[:, 0:N], AF.Sqrt, [rn], [rn])
            k.recip(rn[:, 0:N], rn[:, 0:N], [rn], [rn])
            k.tt(kk[:, 0:N], kk[:, 0:N], rn[:, 0:N], ALU.mult, [kk, rn], [kk])
            k.ts(kp[:, 0:N], av[:, 0:N], pv(3), ALU.mult, [av, pvec], [kp], s2=pv(NPV), op1=ALU.add)
            k.tt(kp[:, 0:N], kp[:, 0:N], U(2 + ct), ALU.mult, [kp, usb], [kp])
            k.tt(bv[:, 0:N], kk[:, 0:N], av[:, 0:N], ALU.mult, [kk, av], [bv], eng="pool")
            k.stt(sq[:, 0:N], U(ct), pv(4), kp[:, 0:N], ALU.mult, ALU.mult, [usb, pvec, kp], [sq])
            k.mm(b1[:, 0:N], bdones, sq[:, 0:N], True, True, [cst, sq], [b1])
            k.tt(bon[:, ct, 0:N], b1[:, 0:N], U(4 + ct), ALU.mult, [b1, usb], [bon])
            k.act(E[:, 0:N], Lc[:, 0:N], AF.Exp, [Lc], [E], scale=-CDEC)
            k.act(Einv[:, 0:N], Lc[:, 0:N], AF.Exp, [Lc], [Einv], scale=CDEC)
            k.tt(tmpa[:, 0:N], Lc[:, 0:N], ld[:, 0:N], ALU.subtract, [Lc, ld], [tmpa], eng="pool")
            k.act(Eprev[:, 0:N], tmpa[:, 0:N], AF.Exp, [tmpa], [Eprev], scale=-CDEC)
            L3 = Lc.t[:, 0:N].rearrange("p (c t) -> p c t", t=64)
            k.tt(rn[:, 0:N].rearrange("p (c t) -> p c t", t=64), L3[:, :, 63:64].to_broadcast([128, nch, 64]), L3, ALU.subtract, [Lc], [rn])
            k.act(Eend[:, 0:N], rn[:, 0:N], AF.Exp, [rn], [Eend], scale=-CDEC)
            k.copy(gam[:, ct, 0:nch], E.t[:, 0:N].rearrange("p (c t) -> p c t", t=64)[:, :, 63], [E], [gam], eng="pool")
            ARv = AR.t[:, ct, 0:nch, :]
            k.stt(ARv[:, :, 0:64], kk[:, 0:N].rearrange("p (c t) -> p c t", t=64), -1.0, Eprev[:, 0:N].rearrange("p (c t) -> p c t", t=64), ALU.mult, ALU.mult, [kk, Eprev], [AR])
            k.tt(ARv[:, :, 64:128], U(ct).rearrange("p (c t) -> p c t", t=64), E[:, 0:N].rearrange("p (c t) -> p c t", t=64), ALU.mult, [usb, E], [AR])
            k.tt(bt[:, ct, 0:N], bv[:, 0:N], Einv[:, 0:N], ALU.mult, [bv, Einv], [bt])
            k.tt(kt_[:, ct, 0:N], kp[:, 0:N], Einv[:, 0:N], ALU.mult, [kp, Einv], [kt_], eng="pool")
            k.tt(Kh[:, ct, 0:N], kp[:, 0:N], Eend[:, 0:N], ALU.mult, [kp, Eend], [Kh])
            k.tt(Bh[:, ct, 0:N], bv[:, 0:N], Eend[:, 0:N], ALU.mult, [bv, Eend], [Bh], eng="pool")
        if DBG.get("rstage", 9) < 3:
            continue
        tmB = [Buf(f"tm{c_}") for c_ in range(8)]

        def st3_gen(c):
            bk = banks[1]
            srcs = [(Kh, 0), (Kh, 1), (Bh, 0), (Bh, 1)]
            for q, (src, ct) in enumerate(srcs):
                k.mm(bk[0:64, q * 128:(q + 1) * 128], src[:, ct, c * 64:(c + 1) * 64], identR, True, True, [src, cst, identb], [bk], inc=(q == 3))
            yield
            k.copy(KhTM[:, c, :], bk[0:64, 0:256], [bk], [tmB[c]], eng="dve")
            k.copy(BhTM[:, c, :], bk[0:64, 256:512], [bk], [tmB[c]], eng="dve")
            for ct in range(2):
                k.mm(bk[0:64, ct * 128:(ct + 1) * 128], usb[:, 4 + ct, UO + c * 64:UO + (c + 1) * 64], ident, True, True, [usb, cst], [bk], inc=(ct == 1))
            yield
            k.copy(Vtm[:, c, :], bk[0:64, 0:256], [bk], [tmB[c]], eng="dve")

        mAB2 = cst.t[0:64, C_MAB4:C_MAB4 + 256].rearrange("p (h c) -> p h c", h=2)
        mC2 = cst.t[0:64, C_MC4:C_MC4 + 128].rearrange("p (h c) -> p h c", h=2)
        id4 = cst.t[0:64, C_ID4:C_ID4 + 256].rearrange("p (h c) -> p h c", h=4)

        def inv_gen(c):
            cp = c % 2
            sa, sbb, x, xb = SA[cp], SB[cp], X[cp], XB[cp]
            cc = slice(c * 64, (c + 1) * 64)
            for h in range(4):
                ct, hp = h // 2, h % 2
                ps_ = slice(hp * 64, hp * 64 + 64)
                k.mm(banks[4 + hp][0:64, ct * 128:(ct + 1) * 128], bt[ps_, ct, cc], AR[ps_, ct, c, :], True, True, [bt, AR], [banks[4 + hp]])
                k.mm(banks[6 + hp][0:64, ct * 128:(ct + 1) * 128], kt_[ps_, ct, cc], AR[ps_, ct, c, :], True, True, [kt_, AR], [banks[6 + hp]])
            yield
            for hp in range(2):
                sav = sa.t[:, :, :].rearrange("p (a b) c -> p a b c", b=2)[:, :, hp, :]
                sbv = sbb.t[:, :, :].rearrange("p (a b) c -> p a b c", b=2)[:, :, hp, :]
                k.tt(sav, banks[4 + hp].t[0:64, 0:256].rearrange("p (a c) -> p a c", a=2), mAB2, ALU.mult, [banks[4 + hp], cst], [sa])
                k.tt(sbv, banks[6 + hp].t[0:64, 0:256].rearrange("p (a c) -> p a c", a=2), mAB2, ALU.mult, [banks[6 + hp], cst], [sbb])
            for h in range(4):
                ct, hp = h // 2, h % 2
                ps_ = slice(hp * 64, hp * 64 + 64)
                k.mm(banks[4 + hp][0:64, ct * 64:(ct + 1) * 64], AR[ps_, ct, c, 0:64], bt[ps_, ct, cc], True, True, [bt, AR], [banks[4 + hp]])
            yield
            pq0 = PQ[0][0]
            k.copy(pq0[:, 0, :, :], sa[:, :, 0:64], [sa], [pq0], eng="dve")
            for hp in range(2):
                k.tt(pq0.t[:, 1, :, :].rearrange("p (a b) c -> p a b c", b=2)[:, :, hp, :], banks[4 + hp].t[0:64, 0:128].rearrange("p (a c) -> p a c", a=2), mC2, ALU.mult, [banks[4 + hp], cst], [pq0])
            k.tt(x[:, :, :], sa[:, :, 0:64], id4, ALU.add, [sa, cst], [x])
            (k.copy(xb[:, :, :], x[:, :, :], [x], [xb], eng=DBG.get("sheng", "dve")) if RD != F32 else None)
            cur = pq0
            bP, bX = banks[6], banks[7]
            for st in range(0, 6):
                nxt = PQ[0][(st + 1) % 2]
                needP = st < 4
                needQ = st < 5
                needX = st >= 1
                last = None
                for h in range(4):
                    if needP:
                        k.mm(bP[0:64, h * 64:(h + 1) * 64], cur[:, 1, h, :], cur[:, 0, h, :], True, True, [cur], [bP], inc=False)
                    if needQ:
                        k.mm(bP[0:64, 256 + h * 64:256 + (h + 1) * 64], cur[:, 0, h, :], cur[:, 1, h, :], True, True, [cur], [bP], inc=(h == 3))
                if needX:
                    for h in range(4):
                        k.mm(bX[0:64, h * 64:(h + 1) * 64], cur[:, 1, h, :], xb[:, h, :], True, True, [cur, xb], [bX], inc=(h == 3))
                yield
                if needP:
                    k.copy(nxt[:, :, :, :], bP.t[0:64, :].rearrange("p (a h c) -> p a h c", a=2, h=4), [bP], [nxt], eng="dve")
                elif needQ:
                    k.copy(nxt[:, 1, :, :], bP.t[0:64, 256:512].rearrange("p (h c) -> p h c", h=4), [bP], [nxt], eng="dve")
                if needX:
                    k.tt(x[:, :, :], bX.t[0:64, 0:256].rearrange("p (h c) -> p h c", h=4), x[:, :, :], ALU.add, [bX, x], [x])
                    (k.copy(xb[:, :, :], x[:, :, :], [x], [xb], eng=DBG.get("sheng", "dve")) if RD != F32 else None)
                cur = nxt
                if st < 5:
                    yield

        def rec_gen(c):
            cp = c % 2
            sa, sbb, x, xb = SA[cp], SB[cp], X[cp], XB[cp]
            cc = slice(c * 64, (c + 1) * 64)
            for hp in range(2):
                k.copy(Vpad.t[:, :, :].rearrange("p (a b) d -> p a b d", b=2)[:, :, hp, hp * 64:(hp + 1) * 64], Vtm.t[:, c, :].rearrange("p (a b d) -> p a b d", a=2, b=2)[:, :, hp, :], [tmB[c]], [Vpad], eng="dve")
            bW, bU, bY, bH = banks[0], banks[0], banks[2], banks[3]
            for h in range(4):
                ct, hp = h // 2, h % 2
                k.mm(bW[0:64, h * 64:(h + 1) * 64], AR[:, ct, c, 0:64], Hb[:, ct, hp * 64:(hp + 1) * 64], True, False, [AR, Hb], [bW], inc=False)
                k.mm(bW[0:64, h * 64:(h + 1) * 64], sbb[:, h, 0:64], Vtm[:, c, h * 64:(h + 1) * 64], False, True, [sbb, tmB[c]], [bW], inc=(h == 3))
            yield
            k.copy(Wsb[:, :], bW[0:64, 0:256], [bW], [Wsb], eng="dve")
            for h in range(4):
                k.mm(bU[0:64, 256 + h * 64:256 + (h + 1) * 64], xb[:, h, :], Wsb[:, h * 64:(h + 1) * 64], True, True, [xb, Wsb], [bU], inc=(h == 3))
            yield
            k.copy(Usb[:, :], bU[0:64, 256:512], [bU], [Usb], eng="dve")
            for hp in range(2):
                k.copy(Upad.t[:, :, :].rearrange("p (a b) d -> p a b d", b=2)[:, :, hp, hp * 64:(hp + 1) * 64], bU.t[0:64, 256:512].rearrange("p (a b d) -> p a b d", a=2, b=2)[:, :, hp, :], [bU], [Upad], eng="dve")
            for ct in range(2):
                o = bY[:, ct * 64:(ct + 1) * 64]
                k.mm(o, Hb[:, ct, :], AR[:, ct, c, 64:128], True, False, [Hb, AR], [bY], inc=False)
                for hp in range(2):
                    h = 2 * ct + hp
                    k.mm(o, Upad[:, h, :], sa[:, h, 64:128], False, False, [Upad, sa], [bY], inc=False)
                    k.mm(o, Vpad[:, h, :], sbb[:, h, 64:128], False, hp == 1, [Vpad, sbb], [bY], inc=(hp == 1 and ct == 1))
            for ct in range(2):
                o = bH[:, ct * 128:(ct + 1) * 128]
                k.mm(o, KhTM[:, c, ct * 128:(ct + 1) * 128], Vtm[:, c, ct * 128:(ct + 1) * 128], True, False, [tmB[c]], [bH], inc=False)
                k.mm(o, BhTM[:, c, ct * 128:(ct + 1) * 128], Usb[:, ct * 128:(ct + 1) * 128], False, True, [tmB[c], Usb], [bH], inc=(ct == 1))
            yield
            k.copy(yT[:, :, cc], bY.t[:, 0:128].rearrange("p (a t) -> p a t", a=2), [bY], [yT], eng="act")
            for ct in range(2):
                for hp in range(2):
                    ps_ = slice(hp * 64, hp * 64 + 64)
                    k.stt(Hbd[ps_, ct, ps_], Hbd[ps_, ct, ps_], gam[ps_, ct, c:c + 1], bH[ps_, ct * 128 + hp * 64:ct * 128 + hp * 64 + 64], ALU.mult, ALU.add, [Hbd, gam, bH], [Hbd])
            (k.copy(Hb[:, :, :], Hbd[:, :, :], [Hbd], [Hb], eng=DBG.get("sheng", "dve")) if RD != F32 else None)

        def drive(gens):
            gens = [g for g in gens if g is not None]
            while gens:
                for g in list(gens):
                    try:
                        next(g)
                    except StopIteration:
                        gens.remove(g)

        drive([st3_gen(0)])
        drive([inv_gen(0), st3_gen(1)])
        for c in range(nch):
            drive([rec_gen(c), inv_gen(c + 1) if c + 1 < nch else None, st3_gen(c + 2) if c + 2 < nch else None])
        if DBG.get("rstage", 9) < 4:
            continue
        for ct in range(2):
            pv = lambda j: pvec[:, ct, j:j + 1]
            b0, b1 = banks[3], banks[4]
            k.mm(b0[:, 0:N], bdones, yT[:, ct, 0:N], True, True, [cst, yT], [b0])
            k.act(sq[:, 0:N], yT[:, ct, 0:N], AF.Square, [yT], [sq])
            k.mm(b1[:, 0:N], bdones, sq[:, 0:N], True, True, [cst, sq], [b1])
            k.ts(kk[:, 0:N], b0[:, 0:N], 1.0 / 64, ALU.mult, [b0], [kk])
            k.tt(rn[:, 0:N], kk[:, 0:N], kk[:, 0:N], ALU.mult, [kk], [rn])
            k.stt(rn[:, 0:N], b1[:, 0:N], 1.0 / 64, rn[:, 0:N], ALU.mult, ALU.subtract, [b1, rn], [rn])
            k.ts(rn[:, 0:N], rn[:, 0:N], GN_EPS, ALU.add, [rn], [rn])
            k.act(rn[:, 0:N], rn[:, 0:N], AF.Sqrt, [rn], [rn])
            k.recip(rn[:, 0:N], rn[:, 0:N], [rn], [rn])
            k.tt(kk[:, 0:N], yT[:, ct, 0:N], kk[:, 0:N], ALU.subtract, [yT, kk], [kk])
            k.tt(kk[:, 0:N], kk[:, 0:N], rn[:, 0:N], ALU.mult, [kk, rn], [kk])
            k.ts(kk[:, 0:N], kk[:, 0:N], pv(5), ALU.mult, [kk, pvec], [kk], s2=pv(6), op1=ALU.add)
            k.tt(kk[:, 0:N], kk[:, 0:N], bon[:, ct, 0:N], ALU.add, [kk, bon], [kk], eng="pool")
            k.tt(ob[:, ct, 0:N], kk[:, 0:N], gv[:, ct, 0:N], ALU.mult, [kk, gv], [ob])
            for t in range(ntl):
                S.dma("sp" if t % 2 == 0 else "act", ob_out(ct, t0 // 128 + t), ob[:, ct, t * 128:(t + 1) * 128], reads=[ob])
    S.barrier()
    es1.close()
    return k, dict(hown=hown, wO_d=wO_d, oa_d=oa_d, cst=cst, identb=identb, ident=ident, kT=kT, kiT=kiT, Vaug=Vaug, banks=banks)


def build_L1_full(k=None, io=None, banks=None):
    alone = k is None
    k, c = build_L1(k, io, banks)
    S = k.S
    hown, wO_d, oa_d = c["hown"], c["wO_d"], c["oa_d"]
    cst, identb, ident = c["cst"], c["identb"], c["ident"]
    kT, kiT, Vaug, banks = c["kT"], c["kiT"], c["Vaug"], c["banks"]
    es2 = ExitStack()
    qT = k.sb("qT", [128, 4, TOWN], BF16, es2)
    qiT = k.sb("qiT", [96, 3, TOWN], BF16, es2)
    wi = k.sb("wi", [128, NOWN, 8], F32, es2)
    wO = k.sb("wO", [128, 8, 776], BF16, es2)
    S.dma("pool", wO[:, :, :], wO_d.rearrange("(kt p) c -> p kt c", p=128), writes=[wO])
    hst = [k.sb("hst2", [128, D], F32, es2)] * 2
    hT = k.sb("hT2", [128, 8, 512], BF16, es2)
    Irows = [k.sb("Irow", [128, LP], F32, es2) for _ in range(2)]
    junk = k.sb("junk", [128, LP], mybir.dt.uint8, es2)
    mbTs = [k.sb("mbT", [128, NT, 256], BF16, es2) for _ in range(2)]
    accS = k.sb("accS", [128, 8, 130], F32, es2)
    loall = k.sb("loall", [128, NOWN], F32, es2)
    Rh = [k.sb("Rh", [128, 512], BF16, es2) for _ in range(8)]
    PTs = [k.sb("PT", [128, 512], BF16, es2) for _ in range(3)]
    Dhs = [k.sb("Dh", [128, 8, 128], BF16, es2) for _ in range(2)]
    m01 = [k.sb("m01", [128, 512], F32, es2) for _ in range(2)]
    oasb = k.sb("oasb", [128, 2, 512], F32, es2)
    st = k.sb("stb", [128, 8], F32, es2)
    Hs = k.sb("Hs", [128, NBIS], F32, es2)
    p2 = k.sb("p2", [128, NBIS], F32, es2)
    rec = k.sb("rec", [128, 8], F32, es2)
    for j in range(NBIS):
        k.memset(p2[:, j:j + 1], float(2.0 ** -(j + 1)), [p2])
    adm = cst.t[:, C_ADM:C_ADM + 384]

    tc = 0
    for g0 in range(0, NOWN, 4):
        nt = min(4, NOWN - g0)
        N = nt * 128
        for t in range(nt):
            hs = hst[tc % 2]; tc += 1
            S.dma("sp" if tc % 2 == 0 else "act", hs[:, :], hown[(g0 + t) * 128:(g0 + t + 1) * 128, :], writes=[hs])
            for half in range(2):
                bk = banks[half]
                for q in range(4):
                    kt = half * 4 + q
                    k.tr(bk[:, q * 128:(q + 1) * 128], hs[:, kt * 128:(kt + 1) * 128], ident, [hs, cst], [bk], inc=(q == 3))
                k.copy(hT[:, half * 4:half * 4 + 4, t * 128:(t + 1) * 128], bk.t[:, :].rearrange("p (q c) -> p q c", q=4), [bk], [hT])
        gc = slice(g0 * 128, g0 * 128 + N)
        for m in range(4):
            bk = banks[2 + m % 4]
            for kt in range(8):
                k.mm(bk[:, 0:N], wO[:, kt, m * 128:(m + 1) * 128], hT[:, kt, 0:N], kt == 0, kt == 7, [wO, hT], [bk], inc=(kt == 7))
            k.copy(qT[:, m, gc], bk[:, 0:N], [bk], [qT])
        for m in range(3):
            M = 96 if m < 2 else 64
            bk = banks[2 + m]
            for kt in range(8):
                k.mm(bk[0:M, 0:N], wO[:, kt, 512 + m * 96:512 + m * 96 + M], hT[:, kt, 0:N], kt == 0, kt == 7, [wO, hT], [bk], inc=(kt == 7))
            k.copy(qiT[0:M, m, gc], bk[0:M, 0:N], [bk], [qiT])
        for t in range(nt):
            bk = banks[6 + t % 2]
            for kt in range(8):
                k.mm(bk[:, 0:8], hT[:, kt, t * 128:(t + 1) * 128], wO[:, kt, 768:776], kt == 0, kt == 7, [hT, wO], [bk], inc=(kt == 7))
            k.copy(wi[:, g0 + t, :], bk[:, 0:8], [bk], [wi])

    def indexer(i):
        Ir, Dh_ = Irows[i % 2], Dhs[i % 2]
        nkb = min(2 * i + 3, NT)
        Si = nkb * 128
        for h in range(8):
            k.ts(Dh_[:, h, :], identb[:, :], wi[:, i, h:h + 1], ALU.mult, [identb, wi], [Dh_], eng="pool")
        for s0 in range(0, Si, 512):
            n = min(512, Si - s0)
            for h in range(8):
                bk = banks[5 + h % 2]
                pb = 32 * (h % 3)
                k.mm(bk[:, 0:n], qiT[pb:pb + 32, h // 3, i * 128:(i + 1) * 128], kiT[pb:pb + 32, s0:s0 + n], True, True, [qiT, kiT], [bk])
                k.act(Rh[h][:, 0:n], bk[:, 0:n], AF.Relu, [bk], [Rh[h]])
            bI = banks[7]
            for h in range(8):
                k.mm(bI[:, 0:n], Dh_[:, h, :], Rh[h][:, 0:n], h == 0, h == 7, [Dh_, Rh[h]], [bI], inc=(h == 7))
            k.copy(Ir[:, s0:s0 + n], bI[:, 0:n], [bI], [Ir], eng="act")

    def bisect(i):
        Ir = Irows[i % 2]
        nkb = min(2 * i + 3, NT)
        Si = nkb * 128
        S.op("dve", lambda e, Si=Si, Ir=Ir: e.tensor_reduce(out=st[:, 0:1], in_=Ir[:, 0:Si], axis=AX.X, op=ALU.max, apply_absolute_value=True), reads=[Ir], writes=[st])
        a0 = 2 * i * 128
        k.tt(Ir[:, a0:Si], Ir[:, a0:Si], adm[:, 0:Si - a0], ALU.add, [Ir, cst], [Ir])
        k.ts(st[:, 1:2], st[:, 0:1], -1.0001, ALU.mult, [st], [st], s2=-1e-6, op1=ALU.add)
        k.ts(st[:, 2:3], st[:, 0:1], 2.0002, ALU.mult, [st], [st], s2=2e-6, op1=ALU.add)
        k.ts(Hs[:, :], p2[:, :], st[:, 2:3], ALU.mult, [p2, st], [Hs])
        nb = DBG.get("nbis", NBIS)
        k.tt(st[:, 3:4], st[:, 1:2], Hs[:, 0:1], ALU.add, [st, Hs], [st])
        for it in range(nb):
            k.ts(junk[:, 0:Si], Ir[:, 0:Si], st[:, 3:4], ALU.is_ge, [Ir, st], [junk, st], op1=ALU.add, accum=st[:, 4:5])
            k.stt(st[:, 5:6], st[:, 4:5], 255.5, Hs[:, it:it + 1], ALU.is_ge, ALU.mult, [st, Hs], [st])
            sub = Hs[:, it + 1:it + 2] if it + 1 < nb else Hs[:, it:it + 1]
            k.stt(st[:, 3:4], st[:, 5:6], st[:, 3:4], sub, ALU.add, ALU.subtract, [st, Hs], [st])
        k.copy(loall[:, i:i + 1], st[:, 3:4] if nb > 0 else st[:, 1:2], [st], [loall], eng="dve")

    def masks(i, p, nkbG, mbT):
        Ir = Irows[i % 2]
        nkb = min(2 * i + 3, NT)
        Si = nkb * 128
        for ci, s0 in enumerate(range(0, Si, 512)):
            n = min(512, Si - s0)
            nq = n // 128
            mt = m01[ci % 2]
            k.ts(mt[:, 0:n], Ir[:, s0:s0 + n], loall[:, i:i + 1], ALU.is_ge, [Ir, loall], [mt])
            bk = banks[5 + ci % 2]
            for q in range(nq):
                k.tr(bk[:, q * 128:(q + 1) * 128], mt[:, q * 128:(q + 1) * 128], ident, [mt, cst], [bk], inc=(q == nq - 1))
            kb0 = s0 // 128
            k.ts(mbT[:, kb0:kb0 + nq, p * 128:(p + 1) * 128], bk.t[:, 0:n].rearrange("p (q c) -> p q c", q=nq), -1.0, ALU.add, [bk], [mbT], s2=-NEG, op1=ALU.mult)
        if nkb < nkbG:
            k.memset(mbT[:, nkb:nkbG, p * 128:(p + 1) * 128], NEG, [mbT])

    G = 2
    groups = [list(range(g0, min(g0 + G, NOWN))) for g0 in range(0, NOWN if DBG.get("attn", 1) else 0, G)]

    def gk(tiles):
        return min(2 * tiles[-1] + 3, NT)

    def heads(gi, tiles, hs):
        mbT = mbTs[gi % 2]
        nt = len(tiles)
        NQ = nt * 128
        nkbG = gk(tiles)
        gc = slice(tiles[0] * 128, tiles[0] * 128 + NQ)
        for h in hs:
            g, hp, hp2 = h // 4, h % 2, h // 2
            ps_ = slice(hp * 64, hp * 64 + 64)
            acc = banks[3 + h % 2]

            def qk(kb):
                bq = banks[kb % 3]
                k.mm(bq[:, 0:NQ], kT[ps_, g, kb * 128:(kb + 1) * 128], qT[ps_, hp2, gc], True, False, [kT, qT], [bq], inc=False)
                k.mm(bq[:, 0:NQ], identb[:, :], mbT[:, kb, 0:NQ], False, True, [identb, mbT], [bq])

            qk(0)
            for kb in range(nkbG):
                if kb + 1 < nkbG:
                    qk(kb + 1)
                bq = banks[kb % 3]
                PT = PTs[kb % 3]
                k.act(PT[:, 0:NQ], bq[:, 0:NQ], AF.Exp, [bq], [PT], scale=0.125)
                for p in range(nt):
                    k.mm(acc[:, p * 65:(p + 1) * 65], PT[:, p * 128:(p + 1) * 128], Vaug[:, kb, g, :], (kb == 0 and p == 0), kb == nkbG - 1, [PT, Vaug], [acc], inc=(p == nt - 1), skip=True)
            k.copy(accS[:, h, 0:nt * 65], acc[:, 0:nt * 65], [acc], [accS], eng="act")

    def finish(tiles):
        nt = len(tiles)
        a4 = accS.t[:, :, 0:nt * 65].rearrange("p h (t c) -> p h t c", c=65)
        for p in range(nt):
            S.op("dve", lambda e, o=rec[:, 0:8], i_=a4[:, :, p, 64]: e.reciprocal(out=o, in_=i_), reads=[accS], writes=[rec])
            k.tt(oasb.t[:, p, :].rearrange("p (h d) -> p h d", h=8), a4[:, :, p, 0:64], rec[:, 0:8].unsqueeze(2).to_broadcast([128, 8, 64]), ALU.mult, [accS, rec], [oasb])
            S.dma("sp", oa_d[tiles[p] * 128:(tiles[p] + 1) * 128, :], oasb[:, p, :], reads=[oasb])

    if groups:
        t0_ = groups[0]
        indexer(t0_[0])
        for n_, i in enumerate(t0_):
            bisect(i)
            if n_ + 1 < len(t0_):
                indexer(t0_[n_ + 1])
        for p, i in enumerate(t0_):
            masks(i, p, gk(t0_), mbTs[0])
    for gi, tiles in enumerate(groups):
        nxt = groups[gi + 1] if gi + 1 < len(groups) else []
        if nxt:
            indexer(nxt[0])
        heads(gi, tiles, range(0, min(4, DBG.get("nheads", 8))))
        if nxt:
            bisect(nxt[0])
            if len(nxt) > 1:
                indexer(nxt[1])
        heads(gi, tiles, range(4, DBG.get("nheads", 8)))
        if len(nxt) > 1:
            bisect(nxt[1])
        finish(tiles)
        for p, i in enumerate(nxt):
            masks(i, p, gk(nxt), mbTs[(gi + 1) % 2])
    S.barrier()
    es2.close()
    if alone:
        return k.done()


def make_cst(half):
    c = np.zeros((128, C_END), np.float32)
    c[:, C_ID:C_ID + 128] = np.eye(128, dtype=np.float32)
    bd = np.zeros((128, 128), np.float32)
    bd[:64, :64] = 1.0
    bd[64:, 64:] = 1.0
    c[:, C_BD:C_BD + 128] = bd
    i = np.arange(64)[:, None]
    t = np.arange(64)[None, :]
    c[:64, C_MAB:C_MAB + 64] = (i < t)
    c[:64, C_MAB + 64:C_MAB + 128] = (i <= t)
    c[:64, C_MC:C_MC + 64] = (t < i)
    rst = np.ones(512, np.float32)
    rst[::64] = 0.0
    c[:, C_RST:C_RST + 512] = rst[None, :]
    a = np.arange(128)
    lim = np.where(a < 16, 16, np.where(a < 80, 80, 144)) + 128 * half
    r = np.arange(384)[None, :]
    c[:, C_ADM:C_ADM + 384] = np.where(r < lim[:, None], 0.0, -1e30)
    for h in range(4):
        c[:64, C_MAB4 + h * 128:C_MAB4 + (h + 1) * 128] = c[:64, C_MAB:C_MAB + 128]
        c[:64, C_MC4 + h * 64:C_MC4 + (h + 1) * 64] = c[:64, C_MC:C_MC + 64]
        c[:64, C_ID4 + h * 64:C_ID4 + (h + 1) * 64] = np.eye(64, dtype=np.float32)
    return c


def prep_L1(l, inp, half):
    w = inp["w_in"][l]
    A0, R0 = 0, 1064
    kc = [w[:, 512 + g * 64:512 + (g + 1) * 64] for g in range(2)]
    ki = w[:, 1024:1056]
    wA = np.concatenate([kc[0], kc[0], kc[1], kc[1], ki, ki, ki, w[:, 640:768]], axis=1)
    own = half * 256
    def rc(off, ct):
        return slice(R0 + off + own + ct * 128, R0 + off + own + (ct + 1) * 128)
    cols = [rc(0, 0), rc(0, 1), rc(512, 0), rc(512, 1), rc(1024, 0), rc(1024, 1), slice(R0 + 1536, R0 + 1664), slice(R0 + 1664, R0 + 1792)]
    wR = np.concatenate([w[:, s] for s in cols], axis=1)
    mu = inp["rwkv_mu"][l]
    muR = np.stack([mu[s.start - R0:s.stop - R0] for s in cols], axis=1)
    qi = w[:, 768:1024]
    wO = np.concatenate([w[:, 0:512], qi, w[:, 1056:1064]], axis=1)
    ch = slice(own, own + 256)
    vecs = [inp["rwkv_w0"][l], inp["rwkv_a0"][l], inp["rwkv_k_k"][l], inp["rwkv_k_a"][l], inp["rwkv_r_k"][l].reshape(-1),
            inp["rwkv_gn_g"][l], inp["rwkv_gn_b"][l]]
    pv = np.stack([v[ch].reshape(2, 128) for v in vecs], axis=-1)
    pvec = np.ascontiguousarray(pv.transpose(1, 0, 2).reshape(128, 2 * NPV))
    return dict(wA=np.ascontiguousarray(wA), wR=np.ascontiguousarray(wR), muR=np.ascontiguousarray(muR),
                wO=np.ascontiguousarray(wO), pvec=pvec,
                wup=np.ascontiguousarray(inp["rwkv_w_up"][l][:, ch]), aup=np.ascontiguousarray(inp["rwkv_a_up"][l][:, ch]),
                gup=np.ascontiguousarray(inp["rwkv_g_up"][l][:, ch]), cst=make_cst(half))


def own_rows(hfull, half):
    t = hfull.reshape(NT, 128, -1)
    return np.ascontiguousarray(t[half::2].reshape(TOWN, -1))


def L2_io(k, sfx=""):
    return dict(wg=k.din("wg" + sfx, [D, 2048]), bg=k.din("bg" + sfx, [128, 16]), wba=k.din("wba" + sfx, [512, D]),
                wbb=k.din("wbb" + sfx, [512, D]), wout=k.din("wout" + sfx, [D, D]), lnp=k.din("lnp" + sfx, [4, D]),
                w1=k.din("w1" + sfx, [16, D, 512]), w3=k.din("w3" + sfx, [16, D, 512]), w2=k.din("w2" + sfx, [16, 512, D]))


def build_L2(k=None, io=None, banks=None):
    alone = k is None
    if alone:
        k = KB()
        io = L2_io(k)
        io.update(hown=k.din("hown", [TOWN, D]), oa=k.din("oa", [TOWN, 512]), obT=k.din("obT", [512, TOWN]),
                  wr=k.din("wr", [D, 16]), br=k.din("br", [1, 16]), idn=k.din("idn", [128, 128]),
                  h1s=k.dout("h1s", [TOWN, D]), out=k.dout("h2", [TOWN, D]), obG=None, selw=None)
    S = k.S
    hown, oa_d, obT_d = io["hown"], io["oa"], io["obT"]
    wg_d, bg_d, wba_d, wbb_d, wout_d, lnp_d = io["wg"], io["bg"], io["wba"], io["wbb"], io["wout"], io["lnp"]
    wr_d, br_d, w1_d, w3_d, w2_d, idn_d = io["wr"], io["br"], io["w1"], io["w3"], io["w2"], io["idn"]
    h1s, out_d, obG, selw_d = io["h1s"], io["out"], io["obG"], io["selw"]

    idn = k.sb("idn", [128, 128]); S.dma("sp", idn[:, :], idn_d[:, :], writes=[idn])
    ident = idn.t[:, :]
    gb1 = k.sb("gb1", [128, 2, D]); gb2 = k.sb("gb2", [128, 2, D])
    for j in range(2):
        S.dma("sp", gb1[:, j, :], lnp_d[j:j + 1, :].to_broadcast([128, D]), writes=[gb1])
        S.dma("sp", gb2[:, j, :], lnp_d[2 + j:3 + j, :].to_broadcast([128, D]), writes=[gb2])
    brb = k.sb("brb", [128, 16]); S.dma("sp", brb[:, :], br_d[0:1, :].to_broadcast([128, 16]), writes=[brb])
    wr = k.sb("wr", [128, 8, 16]); S.dma("sp", wr[:, :, :], wr_d.rearrange("(kt p) c -> p kt c", p=128), writes=[wr])
    bg = k.sb("bg", [128, 16]); S.dma("sp", bg[:, :], bg_d[:, :], writes=[bg])
    h1T = k.sb("h1T", [128, 8, TOWN], BF16)
    comb = k.sb("comb", [128, NOWN, 16])
    if banks is None:
        banks = [k.ps(f"bank{i}", [128, 512]) for i in range(8)]
    if obG is not None:
        selw = k.sb("selw", [128, 2]); S.dma("sp", selw[:, :], selw_d[:, :], writes=[selw])
    xt = k.sb("xt", [128, D]); tmp = k.sb("tmp", [128, D]); ot = k.sb("ot", [128, D]); stt_ = k.sb("stt", [128, 4, 6])

    esA = ExitStack()
    wg = k.sb("wg", [128, 8, 2048], BF16, esA); S.dma("pool", wg[:, :, :], wg_d.rearrange("(kt p) c -> p kt c", p=128), writes=[wg])
    wba = k.sb("wba", [128, 4, D], BF16, esA); S.dma("pool", wba[:, :, :], wba_d.rearrange("(kt p) c -> p kt c", p=128), writes=[wba])
    wbb = k.sb("wbb", [128, 4, D], BF16, esA); S.dma("pool", wbb[:, :, :], wbb_d.rearrange("(kt p) c -> p kt c", p=128), writes=[wbb])
    wout = k.sb("wout", [128, 8, D], BF16, esA); S.dma("pool", wout[:, :, :], wout_d.rearrange("(kt p) c -> p kt c", p=128), writes=[wout])
    hst = k.sb("hst", [128, 4, D], F32, esA)
    oast = k.sb("oast", [128, 512], F32, esA)
    hT = k.sb("hT", [128, 8, 512], BF16, esA)
    oaT = k.sb("oaT", [128, 4, 512], BF16, esA)
    obT = k.sb("obT", [128, 4, 512], BF16, esA)
    obc = [k.sb("obc", [128, 4, 512], BF16, esA) for _ in range(2)] if obG is not None else None
    gT = k.sb("gT", [128, 16, 512], BF16, esA)
    mT = k.sb("mT", [128, 8, 512], BF16, esA)
    t1 = k.sb("t1", [128, 512], F32, esA); t2 = k.sb("t2", [128, 512], F32, esA)
    h1f = k.sb("h1f", [128, 8, 128], F32, esA)
    rs = k.sb("rs", [128, 64], F32, esA)
    pad8 = k.sb("pad8", [128, 4, 8], F32, esA); k.memset(pad8[:, :, :], -1e30, [pad8])
    m8 = k.sb("m8", [128, 4, 8], F32, esA)
    for g0 in range(0, NOWN, 4):
        nt = min(4, NOWN - g0)
        N = nt * 128
        gc = slice(g0 * 128, g0 * 128 + N)
        for t in range(nt):
            S.dma("sp", hst[:, t, :], hown[(g0 + t) * 128:(g0 + t + 1) * 128, :], writes=[hst])
            for half in range(2):
                bk = banks[half]
                for q in range(4):
                    kt = half * 4 + q
                    k.tr(bk[:, q * 128:(q + 1) * 128], hst[:, t, kt * 128:(kt + 1) * 128], ident, [hst, idn], [bk], inc=(q == 3))
                k.copy(hT[:, half * 4:half * 4 + 4, t * 128:(t + 1) * 128], bk.t[:, :].rearrange("p (q c) -> p q c", q=4), [bk], [hT], eng="dve")
            S.dma("act", oast[:, :], oa_d[(g0 + t) * 128:(g0 + t + 1) * 128, :], writes=[oast])
            bk = banks[2]
            for q in range(4):
                k.tr(bk[:, q * 128:(q + 1) * 128], oast[:, q * 128:(q + 1) * 128], ident, [oast, idn], [bk], inc=(q == 3))
            k.copy(oaT[:, :, t * 128:(t + 1) * 128], bk.t[:, :].rearrange("p (q c) -> p q c", q=4), [bk], [oaT], eng="dve")
        if obG is None:
            S.dma("pool", obT[:, :, 0:N], obT_d[:, gc].rearrange("(kt p) c -> p kt c", p=128), writes=[obT])
        else:
            for r_ in range(2):
                for R in range(2):
                    for s_ in range(2):
                        S.dma("pool", obc[r_][s_ * 64:(s_ + 1) * 64, 2 * R:2 * R + 2, 0:N], obG[r_, :, s_, R, :, gc].rearrange("ct w c -> w ct c"), writes=[obc[r_]])
            k.ts(obc[0][:, :, 0:N], obc[0][:, :, 0:N], selw[:, 0:1], ALU.mult, [obc[0], selw], [obc[0]])
            k.stt(obT[:, :, 0:N], obc[1][:, :, 0:N], selw[:, 1:2], obc[0][:, :, 0:N], ALU.mult, ALU.add, [obc[1], selw, obc[0]], [obT])
        for m in range(16):
            bk = banks[3 + m % 2]
            for kt in range(8):
                k.mm(bk[:, 0:N], wg[:, kt, m * 128:(m + 1) * 128], hT[:, kt, 0:N], kt == 0, kt == 7, [wg, hT], [bk], inc=(kt == 7))
            k.act(gT[:, m, 0:N], bk[:, 0:N], AF.Sigmoid, [bk, bg], [gT], bias=bg[:, m:m + 1])
        for m in range(8):
            ba, bb = banks[5], banks[6]
            for kt in range(4):
                k.mm(ba[:, 0:N], wba[:, kt, m * 128:(m + 1) * 128], oaT[:, kt, 0:N], kt == 0, kt == 3, [wba, oaT], [ba], inc=(kt == 3))
            for kt in range(4):
                k.mm(bb[:, 0:N], wbb[:, kt, m * 128:(m + 1) * 128], obT[:, kt, 0:N], kt == 0, kt == 3, [wbb, obT], [bb], inc=(kt == 3))
            k.tt(t1[:, 0:N], ba[:, 0:N], gT[:, m, 0:N], ALU.mult, [ba, gT], [t1])
            k.tt(t2[:, 0:N], bb[:, 0:N], gT[:, 8 + m, 0:N], ALU.mult, [bb, gT], [t2])
            k.tt(mT[:, m, 0:N], t1[:, 0:N], t2[:, 0:N], ALU.add, [t1, t2], [mT], eng="pool")
        for t in range(nt):
            i = g0 + t
            for half in range(2):
                bk = banks[half]
                for kt in range(8):
                    k.mm(bk[:, :], mT[:, kt, t * 128:(t + 1) * 128], wout[:, kt, half * 512:(half + 1) * 512], kt == 0, kt == 7, [mT, wout], [bk], inc=(kt == 7))
                k.stt(xt[:, half * 512:(half + 1) * 512], hst[:, t, half * 512:(half + 1) * 512], ALPHA, bk[:, :], ALU.mult, ALU.add, [hst, bk], [xt])
            layer_norm_tile(k, xt, gb1, ot, tmp, stt_)
            S.dma("sp", h1s[i * 128:(i + 1) * 128, :], ot[:, :], reads=[ot])
            for half in range(2):
                bk = banks[2 + half]
                for q in range(4):
                    kt = half * 4 + q
                    k.tr(bk[:, q * 128:(q + 1) * 128], ot[:, kt * 128:(kt + 1) * 128], ident, [ot, idn], [bk], inc=(q == 3))
                k.copy(h1T[:, half * 4:half * 4 + 4, i * 128:(i + 1) * 128], bk.t[:, :].rearrange("p (q c) -> p q c", q=4), [bk], [h1T], eng="dve")
                k.copy(h1f[:, half * 4:half * 4 + 4, :], bk.t[:, :].rearrange("p (q c) -> p q c", q=4), [bk], [h1f], eng="dve")
            bk = banks[7]
            for kt in range(8):
                k.mm(bk[:, 0:16], h1f[:, kt, :], wr[:, kt, :], kt == 0, kt == 7, [h1f, wr], [bk], inc=(kt == 7))
            sg = rs.t[:, 0:16]; sel = rs.t[:, 16:32]
            k.act(sg, bk[:, 0:16], AF.Sigmoid, [bk], [rs])
            k.tt(sel, sg, brb[:, :], ALU.add, [rs, brb], [rs])
            k.copy(pad8[:, :, 0:4], sel.rearrange("p (g e) -> p g e", g=4), [rs], [pad8], eng="dve")
            for g in range(4):
                S.op("dve", lambda e, o=m8[:, g, :], i_=pad8[:, g, :]: e.max(out=o, in_=i_), reads=[pad8], writes=[m8])
            gsc = rs.t[:, 32:36]; gmx = rs.t[:, 36:37]; goh = rs.t[:, 40:44]; msk = rs.t[:, 44:60]; den = rs.t[:, 60:61]
            k.tt(gsc, m8[:, :, 0], m8[:, :, 1], ALU.add, [m8], [rs])
            S.op("dve", lambda e, o=gmx, i_=gsc: e.tensor_reduce(out=o, in_=i_, axis=AX.X, op=ALU.max), reads=[rs], writes=[rs])
            k.ts(goh, gsc, gmx, ALU.is_ge, [rs], [rs])
            msk3 = msk.rearrange("p (g e) -> p g e", g=4)
            k.tt(msk3, sel.rearrange("p (g e) -> p g e", g=4), m8[:, :, 1:2].to_broadcast([128, 4, 4]), ALU.is_ge, [rs, m8], [rs])
            k.tt(msk3, msk3, goh.unsqueeze(2).to_broadcast([128, 4, 4]), ALU.mult, [rs], [rs])
            k.tt(msk, msk, sg, ALU.mult, [rs], [rs])
            S.op("dve", lambda e, o=den, i_=msk: e.tensor_reduce(out=o, in_=i_, axis=AX.X, op=ALU.add), reads=[rs], writes=[rs])
            k.recip(den, den, [rs], [rs])
            k.ts(comb[:, i, :], msk, den, ALU.mult, [rs], [comb])
    S.barrier()
    esA.close()

    esB = ExitStack()
    yacc = k.sb("yacc", [128, NOWN, D], F32, esB)
    k.memset(yacc[:, :, :], 0.0, [yacc])
    w1s = [k.sb("w1s", [128, 8, 512], BF16, esB) for _ in range(2)]
    w3s = [k.sb("w3s", [128, 8, 512], BF16, esB) for _ in range(2)]
    w2s = [k.sb("w2s", [128, 4, D], BF16, esB) for _ in range(2)]
    z = k.sb("z", [128, 4, 512], BF16, esB)
    sl = k.sb("sl", [128, 512], F32, esB)
    for e_ in range(16):
        p = e_ % 2
        S.dma("pool", w1s[p][:, :, :], w1_d[e_].rearrange("(kt p) c -> p kt c", p=128), writes=[w1s[p]])
        S.dma("pool", w3s[p][:, :, :], w3_d[e_].rearrange("(kt p) c -> p kt c", p=128), writes=[w3s[p]])
        S.dma("pool", w2s[p][:, :, :], w2_d[e_].rearrange("(kt p) c -> p kt c", p=128), writes=[w2s[p]])
        for g0 in range(0, NOWN, 4):
            nt = min(4, NOWN - g0)
            N = nt * 128
            gc = slice(g0 * 128, g0 * 128 + N)
            for m in range(4):
                ba, bc = banks[(2 * m) % 4], banks[(2 * m) % 4 + 1]
                for kt in range(8):
                    k.mm(ba[:, 0:N], w1s[p][:, kt, m * 128:(m + 1) * 128], h1T[:, kt, gc], kt == 0, kt == 7, [w1s[p], h1T], [ba], inc=(kt == 7))
                for kt in range(8):
                    k.mm(bc[:, 0:N], w3s[p][:, kt, m * 128:(m + 1) * 128], h1T[:, kt, gc], kt == 0, kt == 7, [w3s[p], h1T], [bc], inc=(kt == 7))
                k.act(sl[:, 0:N], ba[:, 0:N], AF.Silu, [ba], [sl])
                k.tt(z[:, m, 0:N], sl[:, 0:N], bc[:, 0:N], ALU.mult, [sl, bc], [z])
            for t in range(nt):
                i = g0 + t
                for half in range(2):
                    bk = banks[4 + (2 * t + half) % 4]
                    for m in range(4):
                        k.mm(bk[:, :], z[:, m, t * 128:(t + 1) * 128], w2s[p][:, m, half * 512:(half + 1) * 512], m == 0, m == 3, [z, w2s[p]], [bk], inc=(m == 3))
                    ya = yacc[:, i, half * 512:(half + 1) * 512]
                    k.stt(ya, bk[:, :], comb[:, i, e_:e_ + 1], ya, ALU.mult, ALU.add, [bk, comb, yacc], [yacc])
            if e_ == 15:
                for t in range(nt):
                    i = g0 + t
                    S.dma("sp", xt[:, :], h1s[i * 128:(i + 1) * 128, :], writes=[xt])
                    k.stt(xt[:, :], xt[:, :], ALPHA, yacc[:, i, :], ALU.mult, ALU.add, [xt, yacc], [xt])
                    layer_norm_tile(k, xt, gb2, ot, tmp, stt_)
                    S.dma("act", out_d[i * 128:(i + 1) * 128, :], ot[:, :], reads=[ot])
    S.barrier()
    esB.close()
    if alone:
        return k.done()


def prep_L2(l, inp):
    w = inp["w_in"][l]
    bgv = inp["b_gate"][l].reshape(16, 128).T
    lnp = np.stack([inp["ln1_g"][l], inp["ln1_b"][l], inp["ln2_g"][l], inp["ln2_b"][l]], axis=0)
    return dict(wg=np.ascontiguousarray(w[:, 2856:4904]), bg=np.ascontiguousarray(bgv),
                wba=np.ascontiguousarray(inp["w_branch_a"][l]), wbb=np.ascontiguousarray(inp["w_branch_b"][l]),
                wout=np.ascontiguousarray(inp["w_out"][l]), lnp=np.ascontiguousarray(lnp),
                wr=np.ascontiguousarray(inp["w_router"]), br=np.ascontiguousarray(inp["b_router"].reshape(1, 16)),
                w1=np.ascontiguousarray(inp["w_exp1"][l]), w3=np.ascontiguousarray(inp["w_exp3"][l]),
                w2=np.ascontiguousarray(inp["w_exp2"][l]), idn=np.eye(128, dtype=np.float32))


PAIRS = [[0, 1], [2, 3], [4, 5], [6, 7]]


def build_fused():
    k = KB()
    S = k.S
    nc = k.nc
    xown = k.din("xown", [TOWN, D])
    lng = k.din("lng", [1, D]); lnb = k.din("lnb", [1, D])
    cst_d = k.din("cst", [128, C_END])
    selw_d = k.din("selw", [128, 2])
    wr_d = k.din("wr", [D, 16]); br_d = k.din("br", [1, 16]); idn_d = k.din("idn", [128, 128])
    io1 = [L1_io(k, str(l)) for l in range(2)]
    io2 = [L2_io(k, str(l)) for l in range(2)]
    out_d = k.dout("out", [TOWN, D])
    hO = nc.dram_tensor("hO_i", [TOWN, D], F32).ap()
    hAg = nc.dram_tensor("hAg_i", [2 * TOWN, D], F32).ap()
    oaS = nc.dram_tensor("oaS_i", [TOWN, 512], F32).ap()
    obX = nc.dram_tensor("obX_i", [2 * 256, TOWN], F32).ap()
    obGt = nc.dram_tensor("obG_i", [2 * 2 * 256, TOWN], F32).ap()
    h1s = nc.dram_tensor("h1s_i", [TOWN, D], F32).ap()
    obX3 = obX.rearrange("(r c) t -> r c t", r=2)
    obG6 = obGt.rearrange("(rp ct s R w) t -> rp ct s R w t", rp=2, ct=2, s=2, R=2, w=64)
    banks = [k.ps(f"bank{i}", [128, 512]) for i in range(8)]

    def phase(fn):
        es = ExitStack()
        k.esd = es
        fn()
        S.barrier()
        es.close()
        k.esd = k.es

    def gather(src, dst, nrows, cr):
        r0 = 0
        while r0 < nrows:
            n = min(cr, nrows - r0)
            S.coll(lambda e, r0=r0, n=n: e.collective_compute("AllGather", op=ALU.bypass, replica_groups=PAIRS,
                                                             ins=[src[r0:r0 + n, :]], outs=[dst[2 * r0:2 * r0 + 2 * n, :]]))
            r0 += n
        S.barrier()

    def hag_row(j):
        r, i = j % 2, j // 2
        q = i // 2
        return (q * 512 + r * 256 + (i % 2) * 128) if q < 8 else (4096 + r * 128)

    phase(lambda: build_L0(k, dict(x=xown, g=lng, b=lnb, y=hO)))
    for l in range(2):
        gather(hO, hAg, TOWN, 256)
        d1 = dict(io1[l])
        d1.update(hown=hO, cst=cst_d, oa=oaS,
                  hin_tile=lambda j: hAg[hag_row(j):hag_row(j) + 128, :],
                  ob_out=lambda ct, j: obX3[j % 2, ct * 128:(ct + 1) * 128, (j // 2) * 128:(j // 2) * 128 + 128])
        phase(lambda: build_L1_full(k, d1, banks))
        gather(obX, obGt, 512, 64)
        d2 = dict(io2[l])
        d2.update(hown=hO, oa=oaS, obT=None, wr=wr_d, br=br_d, idn=idn_d, h1s=h1s,
                  out=(out_d if l == 1 else hO), obG=obG6, selw=selw_d)
        phase(lambda: build_L2(k, d2, banks))
    return k.done()


_CACHE = {}


def kernel(**inp):
    inp = {k_: np.asarray(v, dtype=np.float32) for k_, v in inp.items()}
    x = inp["x"]
    B = x.shape[0]
    cores = list(range(8))
    full = np.zeros((B, LP, D), np.float32)
    full[:, :16] = inp["meta_tokens"][None]
    full[:, 16:LR] = x
    if "F" not in _CACHE:
        _CACHE["F"] = build_fused()
    p1 = [[prep_L1(l, inp, half) for half in range(2)] for l in range(2)]
    p2 = [prep_L2(l, inp) for l in range(2)]
    maps = []
    for c in cores:
        b, half = c // 2, c % 2
        m = dict(xown=own_rows(full[b], half), lng=np.ascontiguousarray(inp["ln_in_g"].reshape(1, D)),
                 lnb=np.ascontiguousarray(inp["ln_in_b"].reshape(1, D)), cst=p1[0][half]["cst"],
                 selw=np.ascontiguousarray(np.tile(np.eye(2, dtype=np.float32)[half][None, :], (128, 1))),
                 wr=p2[0]["wr"], br=p2[0]["br"], idn=p2[0]["idn"])
        for l in range(2):
            for nm in ("wA", "wR", "muR", "wO", "pvec", "wup", "aup", "gup"):
                m[nm + str(l)] = p1[l][half][nm]
            for nm in ("wg", "bg", "wba", "wbb", "wout", "lnp", "w1", "w3", "w2"):
                m[nm + str(l)] = p2[l][nm]
        maps.append(m)
    res = run_bass_kernel_spmd(_CACHE["F"], maps, core_ids=cores).results
    h = np.zeros((B, LP, D), np.float32)
    for c in cores:
        b, half = c // 2, c % 2
        h[b].reshape(NT, 128, D)[half::2] = res[c]["out"].reshape(NOWN, 128, D)
    return np.ascontiguousarray(h[:, 16:LR])
```
